# Optimizing a Trainium2 kernel written in Bass

```python
import jax, jax.numpy as jnp
from jax import lax
import numpy as np

D_MODEL = 4096
BATCH = 8
SEQ = 2048
DEPTH = 4

CHUNK = 64
MIX_WIDTH = D_MODEL
CONV_WIDTH = MIX_WIDTH // 2
CONV_K = 3
GLA_WIDTH = MIX_WIDTH - CONV_WIDTH
GLA_HEADS = 4
GLA_KEY_WIDTH = GLA_WIDTH // 2
GLA_DK = GLA_KEY_WIDTH // GLA_HEADS
GLA_DV = GLA_WIDTH // GLA_HEADS
GLA_GATE_RANK = 16
GLA_GATE_TAU = 16.0
D_FF = -(-8 * D_MODEL // (3 * 256)) * 256
NORM_EPS = 1e-6

IN_SPLITS = (CONV_WIDTH, CONV_WIDTH, CONV_WIDTH,
             GLA_KEY_WIDTH, GLA_KEY_WIDTH,
             GLA_WIDTH, GLA_WIDTH,
             GLA_GATE_RANK)
IN_COLS = sum(IN_SPLITS)

kernel_name = "hybrid_conv_gla_parallel_trunk"


def rmsnorm(x, g):
    xf = x.astype(jnp.float32)
    y = xf * lax.rsqrt(jnp.mean(xf * xf, axis=-1, keepdims=True) + NORM_EPS)
    return y.astype(x.dtype) * g


def short_conv_mixer(b, c, h, w_conv):
    u = c * h
    y = lax.conv_general_dilated(
        u, w_conv[:, None, :].astype(u.dtype), window_strides=(1,),
        padding=[(CONV_K - 1, 0)], dimension_numbers=("NWC", "WIO", "NWC"),
        feature_group_count=CONV_WIDTH)
    return b * y


def gla_mixer(q, k, v, g, a_low, w_a_up, b_a, norm_g):
    bsz, s, _ = q.shape
    n = s // CHUNK
    f32 = jnp.float32
    log_a = jax.nn.log_sigmoid((a_low @ w_a_up + b_a).astype(f32)) / GLA_GATE_TAU

    def heads(t, d):
        return t.astype(f32).reshape(bsz, n, CHUNK, GLA_HEADS, d).transpose(1, 0, 3, 2, 4)

    qh = heads(q, GLA_DK) * (GLA_DK ** -0.5)
    kh = heads(k, GLA_DK)
    vh = heads(v, GLA_DV)
    cum = jnp.cumsum(heads(log_a, GLA_DK), axis=3)
    total = cum[:, :, :, -1, :]
    q_dec = qh * jnp.exp(cum)
    k_inv = kh * jnp.exp(-cum)
    k_dec = kh * jnp.exp(total[:, :, :, None, :] - cum)

    mask = jnp.tril(jnp.ones((CHUNK, CHUNK), dtype=bool))
    scores = jnp.einsum("nbhid,nbhjd->nbhij", q_dec, k_inv)
    scores = jnp.where(mask, scores, 0.0)
    o_intra = jnp.einsum("nbhij,nbhjv->nbhiv", scores, vh)

    def step(state, xs):
        qd, kd, vc, tot = xs
        o = jnp.einsum("bhid,bhdv->bhiv", qd, state)
        state = jnp.exp(tot)[..., None] * state + jnp.einsum("bhjd,bhjv->bhdv", kd, vc)
        return state, o

    state0 = jnp.zeros((bsz, GLA_HEADS, GLA_DK, GLA_DV), f32)
    _, o_inter = lax.scan(step, state0, (q_dec, k_dec, vh, total))

    o = (o_intra + o_inter).transpose(1, 0, 3, 2, 4).reshape(bsz, s, GLA_HEADS, GLA_DV)
    o = o * lax.rsqrt(jnp.mean(o * o, axis=-1, keepdims=True) + NORM_EPS)
    o = o * norm_g.reshape(GLA_HEADS, GLA_DV).astype(f32)
    o = o.reshape(bsz, s, GLA_WIDTH) * jax.nn.silu(g.astype(f32))
    return o.astype(q.dtype)


def setup_inputs(seed: int = 0) -> dict:
    key = jax.random.key(seed)
    ks = jax.random.split(key, 16)
    f32 = jnp.float32

    def nrm(k, shape, scale):
        return jax.random.normal(k, shape, f32) * scale

    return {
        "x": nrm(ks[0], (BATCH, SEQ, D_MODEL), 1.0),
        "mix_norm": 1.0 + nrm(ks[1], (DEPTH, D_MODEL), 0.01),
        "w_in": nrm(ks[2], (DEPTH, D_MODEL, IN_COLS), D_MODEL ** -0.5),
        "conv_w": nrm(ks[3], (DEPTH, CONV_K, CONV_WIDTH), CONV_K ** -0.5),
        "gla_a_up": nrm(ks[4], (DEPTH, GLA_GATE_RANK, GLA_KEY_WIDTH), GLA_GATE_RANK ** -0.5),
        "gla_a_bias": nrm(ks[5], (DEPTH, GLA_KEY_WIDTH), 0.1),
        "gla_norm": 1.0 + nrm(ks[6], (DEPTH, GLA_WIDTH), 0.01),
        "w_out": nrm(ks[7], (DEPTH, MIX_WIDTH, D_MODEL), MIX_WIDTH ** -0.5),
        "ffn_norm": 1.0 + nrm(ks[8], (DEPTH, D_MODEL), 0.01),
        "w_gate": nrm(ks[9], (DEPTH, D_MODEL, D_FF), D_MODEL ** -0.5),
        "w_up": nrm(ks[10], (DEPTH, D_MODEL, D_FF), D_MODEL ** -0.5),
        "w_down": nrm(ks[11], (DEPTH, D_FF, D_MODEL), D_FF ** -0.5),
        "final_norm": 1.0 + nrm(ks[12], (D_MODEL,), 0.01),
    }


def reference(x, mix_norm, w_in, conv_w, gla_a_up, gla_a_bias, gla_norm, w_out,
              ffn_norm, w_gate, w_up, w_down, final_norm):
    offsets = np.cumsum(IN_SPLITS)[:-1].tolist()
    for l in range(DEPTH):
        hn = rmsnorm(x, mix_norm[l])
        proj = hn @ w_in[l]
        cb, cc, ch, q, k, v, g, a_low = jnp.split(proj, offsets, axis=-1)
        y_conv = short_conv_mixer(cb, cc, ch, conv_w[l])
        y_gla = gla_mixer(q, k, v, g, a_low, gla_a_up[l], gla_a_bias[l], gla_norm[l])
        x = x + jnp.concatenate([y_conv, y_gla], axis=-1) @ w_out[l]
        hn = rmsnorm(x, ffn_norm[l])
        x = x + (jax.nn.silu(hn @ w_gate[l]) * (hn @ w_up[l])) @ w_down[l]
    return rmsnorm(x, final_norm)
```

```python
import os
import numpy as np
from contextlib import ExitStack
import concourse.bass as bass
import concourse.mybir as mybir
from concourse.bass_utils import run_bass_kernel_spmd

F32 = mybir.dt.float32
BF16 = mybir.dt.bfloat16
AF = mybir.ActivationFunctionType
ALU = mybir.AluOpType

D = 4096
S = 2048
DEPTH = 4
CONVW = 2048
KEYW = 1024
GLAW = 2048
HEADS = 4
DK = 256
DV = 512
RANK = 16
DFF = 11008
INCOLS = 12304
EPS = 1e-6
KC = D // 128
TH = 1024
NHALF = S // TH
O_CB, O_CC, O_CH, O_Q, O_K, O_V, O_G, O_A = 0, 2048, 4096, 6144, 7168, 8192, 10240, 12288


class Buf:
    __slots__ = ("w", "r", "name")

    def __init__(self, name=""):
        self.w = None
        self.r = {}
        self.name = name


class Sem:
    __slots__ = ("h", "n")

    def __init__(self, h):
        self.h = h
        self.n = 0


class Prog:
    ENG = ("sp", "act", "dve", "pool", "pe")
    NSLOT = 8

    def __init__(self, nc, es):
        self.nc = nc
        self.q = {e: [] for e in self.ENG}
        self.esem = {e: Sem(es.enter_context(nc.semaphore("e_" + e))) for e in self.ENG}
        self.dsem = {e: [Sem(es.enter_context(nc.semaphore(f"d_{e}{i}"))) for i in range(self.NSLOT)]
                     for e in ("sp", "pool", "act")}
        self.dcnt = {e: 0 for e in ("sp", "pool", "act")}
        self.out_toks = []

    def _waits(self, eng, R, W):
        ws = {}

        def add(tok):
            if tok is None:
                return
            s, v = tok
            if ws.get(s, (None, -1))[1] < v:
                ws[s] = (s, v)
        for b in R:
            add(b.w)
        for b in W:
            add(b.w)
            for s, v in b.r.items():
                add((s, v))
        if eng == "pe":
            ws.pop(self.esem["pe"], None)
        return list(ws.values())

    def _commit(self, tok, R, W):
        s, v = tok
        for b in R:
            if b.r.get(s, -1) < v:
                b.r[s] = v
        for b in W:
            b.w = tok
            b.r = {}

    def op(self, eng, fns, R=(), W=()):
        if not isinstance(fns, (list, tuple)):
            fns = [fns]
        waits = self._waits(eng, R, W)
        sem = self.esem[eng]
        sem.n += 1
        tok = (sem, sem.n)
        self.q[eng].append((list(fns), waits, sem, 1))
        self._commit(tok, R, W)
        return tok

    def dma(self, eng, fn, R=(), W=(), is_output=False):
        waits = self._waits(eng, R, W)
        i = self.dcnt[eng]
        self.dcnt[eng] += 1
        sem = self.dsem[eng][i % self.NSLOT]
        if sem.n > 0:
            waits.append((sem, sem.n))
        sem.n += 16
        tok = (sem, sem.n)
        self.q[eng].append(([fn], waits, sem, 16))
        self._commit(tok, R, W)
        if is_output:
            self.out_toks.append(tok)
        return tok

    def finish(self):
        waits = [(s, s.n) for s in self.esem.values() if s.n > 0]
        for e in self.dsem:
            waits += [(s, s.n) for s in self.dsem[e] if s.n > 0]
        self.q["sp"].append(([I("nop", )], waits, None, 0))

    def emit(self):
        nc = self.nc
        engs = {"sp": "sync", "act": "scalar", "dve": "vector", "pool": "gpsimd", "pe": "tensor"}

        def run(name, e):
            seen = {}
            for fns, waits, sem, inc in self.q[name]:
                for s, v in waits:
                    if seen.get(s, -1) >= v:
                        continue
                    seen[s] = v
                    e.wait_ge(s.h, v)
                ins = None
                for f in fns:
                    ins = f(e)
                if sem is not None:
                    ins.then_inc(sem.h, inc)
        with nc.Block() as block:
            for name in self.ENG:
                getattr(block, engs[name])(lambda e, name=name: run(name, e))


class Ctx:
    pass


def I(method, *a, **k):
    return lambda e: getattr(e, method)(*a, **k)


def build_program(layers, first, last, nlayers_in, NB=1):
    nc = bass.Bass("TRN2", target_bir_lowering=False)
    L = nlayers_in
    dt = nc.dram_tensor
    if first:
        x_in = dt("x", [NB, S, D], F32, kind="ExternalInput")
    else:
        x_in = dt("xT_in", [KC, 128, S], F32, kind="ExternalInput")
    mix_norm = dt("mix_norm", [L, D], F32, kind="ExternalInput")
    w_in = dt("w_in", [L, D, INCOLS], F32, kind="ExternalInput")
    conv_w = dt("conv_w", [L, 3, CONVW], F32, kind="ExternalInput")
    a_up = dt("gla_a_up", [L, RANK, KEYW], F32, kind="ExternalInput")
    a_bias = dt("gla_a_bias", [L, 1, KEYW], F32, kind="ExternalInput")
    gla_norm = dt("gla_norm", [L, 1, GLAW], F32, kind="ExternalInput")
    w_out = dt("w_out", [L, D, D], F32, kind="ExternalInput")
    ffn_norm = dt("ffn_norm", [L, D], F32, kind="ExternalInput")
    w_gate = dt("w_gate", [L, D, DFF], F32, kind="ExternalInput")
    w_up = dt("w_up", [L, D, DFF], F32, kind="ExternalInput")
    w_down = dt("w_down", [L, DFF, D], F32, kind="ExternalInput")
    final_norm = dt("final_norm", [1, D], F32, kind="ExternalInput")
    consts = dt("consts", [128, 3, 128], F32, kind="ExternalInput")
    if last:
        out = dt("out", [NB, S, D], F32, kind="ExternalOutput")
    else:
        out = dt("xT_out", [KC, 128, S], F32, kind="ExternalOutput")
    kd = "ExternalOutput" if os.environ.get("KDEBUG") else "Internal"
    xT = dt("xT_s", [KC, 128, S], F32, kind=kd)
    PT = dt("PT_s", [96, 128, S], BF16, kind=kd)
    AL = dt("AL_s", [RANK, S], F32, kind=kd)
    KVtm = dt("KV_s", [S, KEYW + GLAW], BF16, kind=kd)
    YT = dt("YT_s", [KC, 128, S], BF16, kind=kd)
    HT = dt("HT_s", [DFF // 128, 128, S], BF16, kind=kd)

    with ExitStack() as es:
        P = Prog(nc, es)
        sb = lambda name, shape, dtype: es.enter_context(nc.sbuf_tensor(name, shape, dtype))
        C = Ctx()
        C.res = sb("res", [128, KC, TH], BF16)
        C.res_b = [Buf(f"res{c}") for c in range(KC)]
        NWB = 2
        C.wb = [sb(f"wb{i}", [128, KC, 256], BF16) for i in range(NWB)]
        C.wb_b = [[Buf(), Buf()] for _ in range(NWB)]
        C.wcnt = 0
        NF = 5
        C.f32 = [sb(f"f32_{i}", [128, TH], F32) for i in range(NF)]
        C.f32_b = [Buf() for _ in range(NF)]
        C.fcnt = 0
        C.rs = sb("rs", [128, TH], F32)
        C.rs_b = Buf()
        NB16 = 6
        C.b16 = [sb(f"b16_{i}", [128, TH], BF16) for i in range(NB16)]
        C.b16_b = [Buf() for _ in range(NB16)]
        C.bcnt = 0
        C.ps = es.enter_context(nc.psum_tensor("ps", [128, 8, 512], F32))
        C.ps_b = [Buf(f"ps{i}") for i in range(8)]
        C.pcnt = 0
        C.ones_bf = sb("ones_bf", [128, 128], BF16)
        C.epsc = sb("epsc", [128, 1], F32)
        C.gains = sb("gains", [128, 3, KC], F32)
        C.gains_b = Buf()
        C.convw = sb("convw", [128, 3, 16], F32)
        C.convw_b = Buf()
        C.const_b = Buf()

        def next_f32():
            i = C.fcnt % NF
            C.fcnt += 1
            return C.f32[i], C.f32_b[i]

        def next_b16():
            i = C.bcnt % NB16
            C.bcnt += 1
            return C.b16[i], C.b16_b[i]

        C.next_f32 = next_f32
        C.next_b16 = next_b16

        C.cst = sb("cst", [128, 3, 128], F32)
        C.ident = C.cst[:, 0, :]
        C.tri = C.cst[:, 1, :]
        C.su = C.cst[:, 2, :]
        C.onec = sb("onec", [128, 1], F32)
        P.dma("sp", I("dma_start", out=C.cst[:, :, :], in_=consts[:, :, :]), W=[C.const_b])

        def setup(e):
            e.memset(C.ones_bf[:, :], 1.0)
            e.memset(C.onec[:, :], 1.0)
            return e.memset(C.epsc[:, :], EPS)
        P.op("pool", setup, W=[C.const_b])
        xT_b = [Buf(f"xT{c}") for c in range(KC)]
        PT_b = [Buf(f"PT{c}") for c in range(96)]
        AL_b = Buf("AL")
        KV_b = [Buf(f"KV{i}") for i in range(12)]
        YT_b = [Buf(f"YT{c}") for c in range(KC)]
        HT_b = [Buf(f"HT{c}") for c in range(DFF // 128)]
        C.xT, C.xT_b, C.PT, C.PT_b, C.AL, C.AL_b = xT, xT_b, PT, PT_b, AL, AL_b
        C.KV, C.KV_b, C.YT, C.YT_b, C.HT, C.HT_b = KVtm, KV_b, YT, YT_b, HT, HT_b

        for b in range(NB):
            if first:
                stage_transpose_in(P, C, x_in[b])
            else:
                for c in range(KC):
                    P.dma("sp", I("dma_start", out=xT[c, :, :], in_=x_in[c, :, :]), W=[xT_b[c]])
            for li in layers:
                stage_load_gains(P, C, mix_norm, ffn_norm, final_norm, conv_w, li)
                stage_inproj(P, C, w_in[li], mix_norm_idx=0)
                side = conv_iter(P, C)
                stage_gla(P, C, a_up[li], a_bias[li], gla_norm[li], es, sb, side=side)
                for _ in side:
                    pass
                stage_gemm_resid(P, C, None, w_out[li], [(0, KC)], src="YT")
                stage_gateup(P, C, w_gate[li], w_up[li])
                stage_gemm_resid(P, C, None, w_down[li], [(0, 29), (29, 29), (58, 28)], src="HT")
            if last:
                stage_final(P, C, out[b])
            else:
                for c in range(KC):
                    P.dma("sp", I("dma_start", out=out[c, :, :], in_=xT[c, :, :]), R=[xT_b[c]], is_output=True)
        P.finish()
        P.emit()
    return nc


def stage_transpose_in(P, C, x_in):
    for tt in range(S // 128):
        for g in range(KC // 8):
            xt, xt_b = C.next_f32()
            P.dma("sp", I("dma_start", out=xt[:, :], in_=x_in[tt * 128:(tt + 1) * 128, g * 1024:(g + 1) * 1024]),
                  W=[xt_b])
            b0 = (C.pcnt % 4) * 2
            C.pcnt += 1
            pbs = [C.ps_b[b0], C.ps_b[b0 + 1]]
            fns = []
            for k in range(8):
                fns.append(I("transpose",
                    C.ps[:, b0 + k // 4, (k % 4) * 128:(k % 4 + 1) * 128], xt[:, k * 128:(k + 1) * 128], C.ident))
            P.op("pe", fns, R=[xt_b, C.const_b], W=pbs)
            ot, ot_b = C.next_f32()
            eng = "act" if (tt * 4 + g) % 2 == 0 else "dve"
            if eng == "act":
                P.op("act", I("activation", out=ot[:, :].rearrange("p (a b) -> p a b", a=2), in_=C.ps[:, b0:b0 + 2, :], func=AF.Copy),
                     R=pbs, W=[ot_b])
            else:
                P.op("dve", I("tensor_copy", out=ot[:, :].rearrange("p (a b) -> p a b", a=2), in_=C.ps[:, b0:b0 + 2, :]),
                     R=pbs, W=[ot_b])
            P.dma("sp", I("dma_start",
                out=C.xT[g * 8:(g + 1) * 8, :, tt * 128:(tt + 1) * 128].rearrange("c p t -> p c t"),
                in_=ot[:, :].rearrange("p (c t) -> p c t", c=8)),
                R=[ot_b], W=[C.xT_b[g * 8 + k] for k in range(8)])


def stage_load_gains(P, C, mix_norm, ffn_norm, final_norm, conv_w, li):
    nc = P.nc
    srcs = [mix_norm[li:li + 1, :], ffn_norm[li:li + 1, :], final_norm[0:1, :]]
    for i, s in enumerate(srcs):
        P.dma("sp", I("dma_start", out=C.gains[:, i, :], in_=s.rearrange("o (c p) -> p (o c)", p=128),
                                                  allow_slow_non_contiguous=True), W=[C.gains_b])
    for k in range(3):
        P.dma("sp", I("dma_start", out=C.convw[:, k, :], in_=conv_w[li, k:k + 1, :].rearrange("o (c p) -> p (o c)", p=128),
                                             allow_slow_non_contiguous=True), W=[C.convw_b])


def norm_to_res(P, C, half, gidx):
    t0 = half * TH
    pbs = [C.ps_b[0], C.ps_b[1]]
    for c in range(KC):
        xs, xs_b = C.next_f32()
        P.dma("sp", I("dma_start", out=xs[:, :], in_=C.xT[c, :, t0:t0 + TH]), R=[C.xT_b[c]], W=[xs_b])
        sq, sq_b = C.next_b16()
        P.op("act", I("activation", out=sq[:, :], in_=xs[:, :], func=AF.Square), R=[xs_b], W=[sq_b])
        fns = [I("matmul", C.ps[:, t, :], C.ones_bf[:, :], sq[:, t * 512:(t + 1) * 512],
                                                  start=(c == 0), stop=(c == KC - 1)) for t in range(2)]
        P.op("pe", fns, R=[sq_b, C.const_b], W=pbs)
    rs, rs_b = C.rs, C.rs_b
    P.op("act", I("activation", out=rs[:, :].rearrange("p (a b) -> p a b", a=2), in_=C.ps[:, 0:2, :], func=AF.Sqrt,
                                              bias=C.epsc[:, 0:1], scale=1.0 / D), R=pbs + [C.const_b], W=[rs_b])
    P.op("dve", I("reciprocal", out=rs[:, :], in_=rs[:, :]), R=[rs_b], W=[rs_b])
    return rs, rs_b


def norm_apply(P, C, half, gidx, rs, rs_b):
    t0 = half * TH
    for c in range(KC):
        xs, xs_b = C.next_f32()
        P.dma("sp", I("dma_start", out=xs[:, :], in_=C.xT[c, :, t0:t0 + TH]), R=[C.xT_b[c]], W=[xs_b])
        eng = "dve"
        P.op(eng, I("scalar_tensor_tensor", out=C.res[:, c, :], in0=xs[:, :], scalar=C.gains[:, gidx, c:c + 1],
                                                              in1=rs[:, :], op0=ALU.mult, op1=ALU.mult),
             R=[xs_b, rs_b, C.gains_b], W=[C.res_b[c]])


def load_w(P, C, w_view, c0, kc, n0, ncols, sub=0):
    i = C.wcnt % len(C.wb)
    wb = C.wb[i]
    if ncols > 128:
        bufs = C.wb_b[i]
        C.wcnt += 1
        P.dma("pool", I("dma_start", out=wb[:, 0:kc, 0:ncols], in_=w_view[:, c0:c0 + kc, n0:n0 + ncols]), W=bufs)
    else:
        bufs = [C.wb_b[i][sub]]
        if sub == 1:
            C.wcnt += 1
        P.dma("pool", I("dma_start", out=wb[:, 0:kc, sub * 128:sub * 128 + ncols], in_=w_view[:, c0:c0 + kc, n0:n0 + ncols]), W=bufs)
    return wb, i


def mm_chunk(P, C, wb, wbuf, woff, m, kc, slot, first_k=True, last_k=True):
    b0 = slot * 2
    fns = []
    for c in range(kc):
        for t in range(2):
            fns.append(I("matmul", C.ps[0:m, b0 + t, :], wb[:, c, woff:woff + m], C.res[:, c, t * 512:(t + 1) * 512],
                                                    start=(first_k and c == 0), stop=(last_k and c == kc - 1)))
    pbs = [C.ps_b[b0], C.ps_b[b0 + 1]]
    P.op("pe", fns, R=[wbuf] + C.res_b[0:kc], W=pbs)
    return b0, pbs


def stage_inproj(P, C, w_l, mix_norm_idx):
    w_view = w_l.rearrange("(c p) n -> p c n", p=128)
    fm_groups = [(n0, 256) for n0 in range(0, O_V, 256)] + [(n0, 256) for n0 in range(O_G, O_A, 256)]
    for half in range(NHALF):
        t0 = half * TH
        rs, rs_b = norm_to_res(P, C, half, mix_norm_idx)
        norm_apply(P, C, half, mix_norm_idx, rs, rs_b)
        for (n0, ncols) in fm_groups:
            wb, wi = load_w(P, C, w_view, 0, KC, n0, ncols)
            for j in range(2):
                col = n0 + j * 128
                pt_idx = col // 128 if col < O_V else (col - O_G) // 128 + 80
                slot = C.pcnt % 4
                C.pcnt += 1
                b0, pbs = mm_chunk(P, C, wb, C.wb_b[wi][j], j * 128, 128, KC, slot)
                ot, ot_b = C.next_b16()
                evac_copy(P, C, b0, pbs, ot, ot_b, 128)
                P.dma("sp", I("dma_start", out=C.PT[pt_idx, :, t0:t0 + TH], in_=ot[:, :]),
                      R=[ot_b], W=[C.PT_b[pt_idx]])
        wb, wi = load_w(P, C, w_view, 0, KC, O_A, RANK, sub=0)
        C.wcnt += 1
        slot = C.pcnt % 4
        C.pcnt += 1
        b0, pbs = mm_chunk(P, C, wb, C.wb_b[wi][0], 0, RANK, KC, slot)
        ot, ot_b = C.next_f32()
        P.op("act", I("activation", out=ot[0:RANK, :].rearrange("p (a b) -> p a b", a=2), in_=C.ps[0:RANK, b0:b0 + 2, :], func=AF.Copy),
             R=pbs, W=[ot_b])
        P.dma("sp", I("dma_start", out=C.AL[:, t0:t0 + TH], in_=ot[0:RANK, :]), R=[ot_b], W=[C.AL_b])
        for gi, n0 in enumerate(range(O_K, O_G, 256)):
            wb, wi = load_w(P, C, w_view, 0, KC, n0, 256)
            st, st_b = C.next_b16()
            for tt in range(TH // 128):
                bank = (C.pcnt % 4) * 2 + (tt % 2)
                if tt % 2 == 1:
                    C.pcnt += 1
                fns = [I("matmul", C.ps[:, bank, 0:256], C.res[:, c, tt * 128:(tt + 1) * 128], wb[:, c, 0:256],
                                                                 start=(c == 0), stop=(c == KC - 1)) for c in range(KC)]
                P.op("pe", fns, R=C.wb_b[wi] + C.res_b, W=[C.ps_b[bank]])
                if tt % 4 == 0 and tt > 0:
                    st, st_b = C.next_b16()
                eng = "act" if tt % 2 == 0 else "dve"
                q4 = tt % 4
                if eng == "act":
                    P.op("act", I("activation", out=st[:, q4 * 256:(q4 + 1) * 256], in_=C.ps[:, bank, 0:256], func=AF.Copy),
                         R=[C.ps_b[bank]], W=[st_b])
                else:
                    P.op("dve", I("tensor_copy", out=st[:, q4 * 256:(q4 + 1) * 256], in_=C.ps[:, bank, 0:256]),
                         R=[C.ps_b[bank]], W=[st_b])
                if q4 == 3:
                    tbase = t0 + (tt - 3) * 128
                    P.dma("sp", I("dma_start",
                        out=C.KV[tbase:tbase + 512, gi * 256:(gi + 1) * 256].rearrange("(a p) n -> p a n", p=128),
                        in_=st[:, :].rearrange("p (a n) -> p a n", a=4)), R=[st_b], W=[C.KV_b[gi]])


def evac_copy(P, C, b0, pbs, ot, ot_b, m):
    C.ecnt = getattr(C, "ecnt", 0) + 1
    o3 = ot[0:m, :].rearrange("p (a b) -> p a b", a=2)
    if C.ecnt % 2 == 0:
        P.op("act", I("activation", out=o3, in_=C.ps[0:m, b0:b0 + 2, :], func=AF.Copy), R=pbs, W=[ot_b])
    else:
        P.op("dve", I("tensor_copy", out=o3, in_=C.ps[0:m, b0:b0 + 2, :]), R=pbs, W=[ot_b])


def conv_iter(P, C):
    for j in range(CONVW // 128):
        for half in range(NHALF):
            t0 = half * TH
            cb, cb_b = C.next_b16()
            cc, cc_b = C.next_b16()
            ch, ch_b = C.next_b16()
            P.dma("sp", I("dma_start", out=cb[:, :], in_=C.PT[j, :, t0:t0 + TH]), R=[C.PT_b[j]], W=[cb_b])
            u, u_b = C.next_f32()
            u2, u2_b = C.next_f32()
            if half == 0:
                P.op("pool", [I("memset", cc[:, 0:2], 0.0), I("memset", ch[:, 0:2], 0.0)], W=[cc_b, ch_b])
                P.dma("sp", I("dma_start", out=cc[:, 2:TH], in_=C.PT[16 + j, :, 0:TH - 2]), R=[C.PT_b[16 + j]], W=[cc_b])
                P.dma("sp", I("dma_start", out=ch[:, 2:TH], in_=C.PT[32 + j, :, 0:TH - 2]), R=[C.PT_b[32 + j]], W=[ch_b])
            else:
                P.dma("sp", I("dma_start", out=cc[:, :], in_=C.PT[16 + j, :, t0 - 2:t0 + TH - 2]), R=[C.PT_b[16 + j]], W=[cc_b])
                P.dma("sp", I("dma_start", out=ch[:, :], in_=C.PT[32 + j, :, t0 - 2:t0 + TH - 2]), R=[C.PT_b[32 + j]], W=[ch_b])
            c2, c2_b = C.next_b16()
            P.dma("sp", I("dma_start", out=c2[:, 0:2], in_=C.PT[16 + j, :, t0 + TH - 2:t0 + TH]), R=[C.PT_b[16 + j]], W=[c2_b])
            P.dma("sp", I("dma_start", out=c2[:, 2:4], in_=C.PT[32 + j, :, t0 + TH - 2:t0 + TH]), R=[C.PT_b[32 + j]], W=[c2_b])
            P.op("pool", I("tensor_tensor", out=u[:, :], in0=cc[:, :], in1=ch[:, :], op=ALU.mult), R=[cc_b, ch_b], W=[u_b])
            P.op("dve", I("tensor_tensor", out=u2[:, 0:2], in0=c2[:, 0:2], in1=c2[:, 2:4], op=ALU.mult), R=[c2_b], W=[u2_b])
            y, y_b = C.next_f32()
            w0 = C.convw[:, 0, j:j + 1]
            w1 = C.convw[:, 1, j:j + 1]
            w2 = C.convw[:, 2, j:j + 1]
            P.op("act", I("activation", out=y[:, :], in_=u[:, :], func=AF.Copy, scale=w0), R=[u_b, C.convw_b], W=[y_b])
            P.op("dve", I("scalar_tensor_tensor", out=y[:, 0:TH - 1], in0=u[:, 1:TH], scalar=w1, in1=y[:, 0:TH - 1], op0=ALU.mult, op1=ALU.add),
                 R=[u_b, C.convw_b, y_b], W=[y_b])
            P.op("dve", I("scalar_tensor_tensor", out=y[:, TH - 1:TH], in0=u2[:, 0:1], scalar=w1, in1=y[:, TH - 1:TH], op0=ALU.mult, op1=ALU.add),
                 R=[u2_b, C.convw_b, y_b], W=[y_b])
            P.op("dve", I("scalar_tensor_tensor", out=y[:, 0:TH - 2], in0=u[:, 2:TH], scalar=w2, in1=y[:, 0:TH - 2], op0=ALU.mult, op1=ALU.add),
                 R=[u_b, C.convw_b, y_b], W=[y_b])
            P.op("dve", I("scalar_tensor_tensor", out=y[:, TH - 2:TH], in0=u2[:, 0:2], scalar=w2, in1=y[:, TH - 2:TH], op0=ALU.mult, op1=ALU.add),
                 R=[u2_b, C.convw_b, y_b], W=[y_b])
            yb, yb_b = C.next_b16()
            P.op("pool", I("tensor_tensor", out=yb[:, :], in0=y[:, :], in1=cb[:, :], op=ALU.mult), R=[y_b, cb_b], W=[yb_b])
            P.dma("sp", I("dma_start", out=C.YT[j, :, t0:t0 + TH], in_=yb[:, :]), R=[yb_b], W=[C.YT_b[j]])
            yield


def stage_gla(P, C, a_up_l, a_bias_l, gnorm_l, es, sb, side=None):
    nc = P.nc
    G = C.gla = getattr(C, "gla", None) or Ctx()
    if not hasattr(G, "init"):
        G.init = True
        def view(ap, a):
            return ap.rearrange("p c t -> p (c t)").rearrange("p (a s) -> p a s", a=a)
        G.qT = view(C.res[:, 0:4, :], 2); G.qT_bs = C.res_b[0:4]
        G.kT = view(C.res[:, 4:8, :], 2); G.kT_bs = C.res_b[4:8]
        G.gT = view(C.res[:, 8:16, :], 4); G.gT_bs = C.res_b[8:16]
        G.vtm = view(C.res[:, 16:24, :], S // 128); G.vtm_bs = C.res_b[16:24]
        G.ktm = view(C.res[:, 24:28, :], S // 128); G.ktm_bs = C.res_b[24:28]
        G.alT = sb("g_alT", [RANK + 1, S], F32); G.alT_b = Buf()
        G.aup = sb("g_aup", [RANK + 1, KEYW], F32); G.aup_b = Buf()
        G.gn = sb("g_gn", [128, 16], F32); G.gn_b = Buf()
        G.state = sb("g_state", [128, 2, DV], F32); G.state_b = Buf()
        G.state16 = sb("g_state16", [128, 2, DV], BF16); G.state16_b = Buf()
        NT = 3
        G.nls = [sb(f"g_nls{i}", [128, DK], F32) for i in range(NT)]; G.nls_b = [Buf() for _ in range(NT)]
        G.e1 = [sb(f"g_e1{i}", [128, 2, 128], F32) for i in range(NT)]; G.e1_b = [Buf() for _ in range(NT)]
        G.e2 = [sb(f"g_e2{i}", [128, 2, 128], F32) for i in range(NT)]; G.e2_b = [Buf() for _ in range(NT)]
        G.etot = [sb(f"g_etot{i}", [128, 2], F32) for i in range(NT)]; G.etot_b = [Buf() for _ in range(NT)]
        G.qd = [sb(f"g_qd{i}", [128, 2, 128], BF16) for i in range(NT)]; G.qd_b = [Buf() for _ in range(NT)]
        G.ki = [sb(f"g_ki{i}", [128, 2, 128], BF16) for i in range(NT)]; G.ki_b = [Buf() for _ in range(NT)]
        G.kd = [sb(f"g_kd{i}", [128, DK], BF16) for i in range(NT)]; G.kd_b = [Buf() for _ in range(NT)]
        G.esuf = [sb(f"g_esuf{i}", [128, DK], F32) for i in range(NT)]; G.esuf_b = [Buf() for _ in range(NT)]
        G.sT = [sb(f"g_sT{i}", [128, 128], BF16) for i in range(NT)]; G.sT_b = [Buf() for _ in range(NT)]
        G.o = [sb(f"g_o{i}", [128, 4, 128], F32) for i in range(NT)]; G.o_b = [Buf() for _ in range(NT)]
        G.sq = [sb(f"g_sq{i}", [128, 4, 128], BF16) for i in range(NT)]; G.sq_b = [Buf() for _ in range(NT)]
        G.rstd = [sb(f"g_rstd{i}", [128, 128], F32) for i in range(NT)]; G.rstd_b = [Buf() for _ in range(NT)]
        G.sg = [sb(f"g_sg{i}", [128, 4, 128], F32) for i in range(NT)]; G.sg_b = [Buf() for _ in range(NT)]
        G.y = view(C.wb[0][:, :, :], 4); G.y_bs = C.wb_b[0]
        G.NT = NT
        G.cnt = 0
    NT = G.NT
    P.dma("sp", I("dma_start", out=G.aup[0:RANK, :], in_=a_up_l[:, :]), W=[G.aup_b])
    P.dma("sp", I("dma_start", out=G.aup[RANK:RANK + 1, :], in_=a_bias_l[:, :]), W=[G.aup_b])
    P.dma("sp", I("dma_start", out=G.gn[:, :], in_=gnorm_l.rearrange("o (c p) -> p (o c)", p=128), allow_slow_non_contiguous=True), W=[G.gn_b])
    P.op("pool", I("memset", G.alT[:, :], 1.0), W=[G.alT_b])
    P.dma("sp", I("dma_start", out=G.alT[0:RANK, :], in_=C.AL[:, :]), R=[C.AL_b], W=[G.alT_b])
    for h in range(HEADS):
        for c in range(2):
            P.dma("sp", I("dma_start", out=G.qT[:, c, :], in_=C.PT[48 + h * 2 + c, :, :]), R=[C.PT_b[48 + h * 2 + c]], W=G.qT_bs)
            P.dma("sp", I("dma_start", out=G.kT[:, c, :], in_=C.PT[56 + h * 2 + c, :, :]), R=[C.PT_b[56 + h * 2 + c]], W=G.kT_bs)
        for c in range(4):
            P.dma("sp", I("dma_start", out=G.gT[:, c, :], in_=C.PT[80 + h * 4 + c, :, :]), R=[C.PT_b[80 + h * 4 + c]], W=G.gT_bs)
        P.dma("sp", I("dma_start", out=G.ktm[:, :, :], in_=C.KV[:, h * DK:(h + 1) * DK].rearrange("(a p) n -> p a n", p=128)),
              R=C.KV_b, W=G.ktm_bs)
        P.dma("sp", I("dma_start", out=G.vtm[:, :, :], in_=C.KV[:, KEYW + h * DV:KEYW + (h + 1) * DV].rearrange("(a p) n -> p a n", p=128)),
              R=C.KV_b, W=G.vtm_bs)
        P.op("pool", [I("memset", G.state[:, :, :], 0.0), I("memset", G.state16[:, :, :], 0.0)], W=[G.state_b, G.state16_b])
        P.op("act", I("activation", out=G.gT[:, :, :], in_=G.gT[:, :, :], func=AF.Silu), R=G.gT_bs, W=G.gT_bs)

        def T(eng, fns, R, W):
            return lambda: P.op(eng, fns, R=R, W=W)

        def phaseA(tt, i):
            tsl = slice(tt * 128, (tt + 1) * 128)
            nls, nls_b = G.nls[i], G.nls_b[i]
            e1, e1_b, e2, e2_b, etot, etot_b = G.e1[i], G.e1_b[i], G.e2[i], G.e2_b[i], G.etot[i], G.etot_b[i]
            esuf, esuf_b = G.esuf[i], G.esuf_b[i]
            qd, qd_b, ki, ki_b, kd, kd_b = G.qd[i], G.qd_b[i], G.ki[i], G.ki_b[i], G.kd[i], G.kd_b[i]
            sT, sT_b = G.sT[i], G.sT_b[i]
            ncum = C.ps[:, 1, 0:256].rearrange("p (c t) -> p c t", c=2)
            st = []
            st.append(T("pe", I("matmul", C.ps[:, 0, 0:DK], G.alT[:, tsl], G.aup[:, h * DK:(h + 1) * DK], start=True, stop=True),
                        [G.alT_b, G.aup_b], [C.ps_b[0]]))
            st.append(T("act", I("activation", out=nls[:, :], in_=C.ps[:, 0, 0:DK], func=AF.Exp, scale=-1.0), [C.ps_b[0]], [nls_b]))
            st.append(T("act", I("activation", out=nls[:, :], in_=nls[:, :], func=AF.Ln, bias=C.onec[:, 0:1], scale=1.0), [nls_b, C.const_b], [nls_b]))
            st.append(T("pe", [I("matmul", C.ps[:, 1, c * 128:(c + 1) * 128], nls[:, c * 128:(c + 1) * 128], C.tri, start=True, stop=True) for c in range(2)],
                        [nls_b, C.const_b], [C.ps_b[1]]))
            st.append(T("pe", I("matmul", C.ps[:, 2, 0:DK], C.su, nls[:, :], start=True, stop=True), [nls_b, C.const_b], [C.ps_b[2]]))
            st.append(T("act", I("activation", out=e1[:, :, :], in_=ncum, func=AF.Exp, scale=-1.0 / 16.0), [C.ps_b[1]], [e1_b]))
            st.append(T("act", I("activation", out=e2[:, :, :], in_=ncum, func=AF.Exp, scale=1.0 / 16.0), [C.ps_b[1]], [e2_b]))
            st.append(T("act", I("activation", out=etot[:, :], in_=ncum[:, :, 127], func=AF.Exp, scale=-1.0 / 16.0), [C.ps_b[1]], [etot_b]))
            st.append(T("act", I("activation", out=esuf[:, :], in_=C.ps[:, 2, 0:DK], func=AF.Exp, scale=-1.0 / 16.0), [C.ps_b[2]], [esuf_b]))
            st.append(T("dve", I("scalar_tensor_tensor", out=qd[:, :, :], in0=G.qT[:, :, tsl], scalar=float(DK) ** -0.5, in1=e1[:, :, :],
                                 op0=ALU.mult, op1=ALU.mult), G.qT_bs + [e1_b], [qd_b]))
            st.append(T("pool", I("tensor_tensor", out=ki[:, :, :], in0=G.kT[:, :, tsl], in1=e2[:, :, :], op=ALU.mult), G.kT_bs + [e2_b], [ki_b]))
            st.append(T("dve", I("tensor_tensor", out=kd[:, :], in0=G.ktm[:, tt, :], in1=esuf[:, :], op=ALU.mult), G.ktm_bs + [esuf_b], [kd_b]))
            st.append(T("pe", [I("matmul", C.ps[:, 3, 0:128], ki[:, c, :], qd[:, c, :], start=(c == 0), stop=(c == 1)) for c in range(2)],
                        [ki_b, qd_b], [C.ps_b[3]]))
            st.append(T("dve", I("tensor_tensor", out=sT[:, :], in0=C.ps[:, 3, 0:128], in1=C.tri, op=ALU.mult), [C.ps_b[3], C.const_b], [sT_b]))
            return st

        def phaseB(tt, i):
            tsl = slice(tt * 128, (tt + 1) * 128)
            etot, etot_b = G.etot[i], G.etot_b[i]
            qd, qd_b, kd, kd_b = G.qd[i], G.qd_b[i], G.kd[i], G.kd_b[i]
            sT, sT_b = G.sT[i], G.sT_b[i]
            o, o_b, sq, sq_b = G.o[i], G.o_b[i], G.sq[i], G.sq_b[i]
            rstd, rstd_b = G.rstd[i], G.rstd_b[i]
            ops4 = C.ps[:, 4, :].rearrange("p (c t) -> p c t", c=4)
            st = []
            fns = []
            for vc in range(4):
                fns.append(I("matmul", C.ps[:, 4, vc * 128:(vc + 1) * 128], G.vtm[:, tt, vc * 128:(vc + 1) * 128], sT[:, :], start=True, stop=False))
                for c in range(2):
                    fns.append(I("matmul", C.ps[:, 4, vc * 128:(vc + 1) * 128], G.state16[:, c, vc * 128:(vc + 1) * 128], qd[:, c, :],
                                 start=False, stop=(c == 1)))
            st.append(T("pe", fns, G.vtm_bs + [sT_b, G.state16_b, qd_b], [C.ps_b[4]]))
            st.append(T("pe", [I("matmul", C.ps[:, 5 + c, :], kd[:, c * 128:(c + 1) * 128], G.vtm[:, tt, :], start=True, stop=True) for c in range(2)],
                        [kd_b] + G.vtm_bs, [C.ps_b[5], C.ps_b[6]]))
            for c in range(2):
                st.append(T("dve", I("scalar_tensor_tensor", out=G.state[:, c, :], in0=G.state[:, c, :], scalar=etot[:, c:c + 1], in1=C.ps[:, 5 + c, :],
                                     op0=ALU.mult, op1=ALU.add), [etot_b, C.ps_b[5 + c], G.state_b], [G.state_b]))
            st.append(T("pool", I("tensor_copy", out=G.state16[:, :, :], in_=G.state[:, :, :]), [G.state_b], [G.state16_b]))
            st.append(T("act", I("activation", out=o[:, :, :], in_=ops4, func=AF.Copy), [C.ps_b[4]], [o_b]))
            st.append(T("act", I("activation", out=sq[:, :, :], in_=ops4, func=AF.Square), [C.ps_b[4]], [sq_b]))
            st.append(T("pe", [I("matmul", C.ps[:, 7, 0:128], C.ones_bf[:, :], sq[:, vc, :], start=(vc == 0), stop=(vc == 3)) for vc in range(4)],
                        [sq_b, C.const_b], [C.ps_b[7]]))
            st.append(T("act", I("activation", out=rstd[:, :], in_=C.ps[:, 7, 0:128], func=AF.Ln, bias=C.epsc[:, 0:1], scale=1.0 / DV),
                        [C.ps_b[7], C.const_b], [rstd_b]))
            st.append(T("act", I("activation", out=rstd[:, :], in_=rstd[:, :], func=AF.Exp, scale=-0.5), [rstd_b], [rstd_b]))
            for vc in range(4):
                st.append(T("dve", I("scalar_tensor_tensor", out=o[:, vc, :], in0=o[:, vc, :], scalar=G.gn[:, h * 4 + vc:h * 4 + vc + 1], in1=rstd[:, :],
                                     op0=ALU.mult, op1=ALU.mult), [o_b, rstd_b, G.gn_b], [o_b]))
            st.append(T("pool", I("tensor_tensor", out=G.y[:, :, tsl], in0=o[:, :, :], in1=G.gT[:, :, tsl], op=ALU.mult), [o_b] + G.gT_bs, G.y_bs))
            return st

        NTT = S // 128
        idx = [(G.cnt + k) % NT for k in range(NTT)]
        G.cnt += NTT
        for th in phaseA(0, idx[0]):
            th()
        for tt in range(NTT):
            a = phaseA(tt + 1, idx[tt + 1]) if tt + 1 < NTT else []
            b = phaseB(tt, idx[tt])
            for k in range(max(len(a), len(b))):
                if k < len(b):
                    b[k]()
                if k < len(a):
                    a[k]()
            if side is not None:
                next(side, None)
        for vc in range(4):
            P.dma("sp", I("dma_start", out=C.YT[16 + h * 4 + vc, :, :], in_=G.y[:, vc, :]), R=G.y_bs, W=[C.YT_b[16 + h * 4 + vc]])


def stage_gemm_resid(P, C, _unused, w_l, kpasses, src):
    w_view = w_l.rearrange("(c p) n -> p c n", p=128)
    srcT = C.YT if src == "YT" else C.HT
    src_b = C.YT_b if src == "YT" else C.HT_b
    for half in range(NHALF):
        t0 = half * TH
        for (c0, kc) in kpasses:
            for c in range(kc):
                P.dma("sp", I("dma_start", out=C.res[:, c, :], in_=srcT[c0 + c, :, t0:t0 + TH]), R=[src_b[c0 + c]], W=[C.res_b[c]])
            for n0 in range(0, D, 256):
                wb, wi = load_w(P, C, w_view, c0, kc, n0, 256)
                for j in range(2):
                    nch = n0 // 128 + j
                    slot = C.pcnt % 4
                    C.pcnt += 1
                    b0, pbs = mm_chunk(P, C, wb, C.wb_b[wi][j], j * 128, 128, kc, slot)
                    xs, xs_b = C.next_f32()
                    P.dma("sp", I("dma_start", out=xs[:, :], in_=C.xT[nch, :, t0:t0 + TH]), R=[C.xT_b[nch]], W=[xs_b])
                    P.op("dve", I("tensor_tensor", out=xs[:, :].rearrange("p (a b) -> p a b", a=2), in0=xs[:, :].rearrange("p (a b) -> p a b", a=2),
                                                                         in1=C.ps[:, b0:b0 + 2, :], op=ALU.add), R=pbs + [xs_b], W=[xs_b])
                    P.dma("sp", I("dma_start", out=C.xT[nch, :, t0:t0 + TH], in_=xs[:, :]), R=[xs_b], W=[C.xT_b[nch]])


def stage_gateup(P, C, wg_l, wu_l):
    wg_view = wg_l.rearrange("(c p) n -> p c n", p=128)
    wu_view = wu_l.rearrange("(c p) n -> p c n", p=128)
    for half in range(NHALF):
        t0 = half * TH
        rs, rs_b = norm_to_res(P, C, half, 1)
        norm_apply(P, C, half, 1, rs, rs_b)
        for j in range(DFF // 128):
            wb, wi = load_w(P, C, wg_view, 0, KC, j * 128, 128, sub=0)
            wb, wi = load_w(P, C, wu_view, 0, KC, j * 128, 128, sub=1)
            slot = C.pcnt % 4
            C.pcnt += 1
            bg, pg = mm_chunk(P, C, wb, C.wb_b[wi][0], 0, 128, KC, slot)
            slot = C.pcnt % 4
            C.pcnt += 1
            bu, pu = mm_chunk(P, C, wb, C.wb_b[wi][1], 128, 128, KC, slot)
            sg, sg_b = C.next_f32()
            P.op("act", I("activation", out=sg[:, :].rearrange("p (a b) -> p a b", a=2), in_=C.ps[:, bg:bg + 2, :], func=AF.Silu), R=pg, W=[sg_b])
            hb, hb_b = C.next_b16()
            P.op("dve", I("tensor_tensor", out=hb[:, :].rearrange("p (a b) -> p a b", a=2), in0=sg[:, :].rearrange("p (a b) -> p a b", a=2),
                                                                        in1=C.ps[:, bu:bu + 2, :], op=ALU.mult), R=pu + [sg_b], W=[hb_b])
            P.dma("sp", I("dma_start", out=C.HT[j, :, t0:t0 + TH], in_=hb[:, :]), R=[hb_b], W=[C.HT_b[j]])


def stage_final(P, C, out):
    for half in range(NHALF):
        t0 = half * TH
        rs, rs_b = norm_to_res(P, C, half, 2)
        for c in range(KC):
            xs, xs_b = C.next_f32()
            P.dma("sp", I("dma_start", out=xs[:, :], in_=C.xT[c, :, t0:t0 + TH]), R=[C.xT_b[c]], W=[xs_b])
            P.op("dve", I("scalar_tensor_tensor", out=xs[:, :], in0=xs[:, :], scalar=C.gains[:, 2, c:c + 1], in1=rs[:, :], op0=ALU.mult, op1=ALU.mult),
                 R=[xs_b, rs_b, C.gains_b], W=[xs_b])
            b0 = (C.pcnt % 4) * 2
            C.pcnt += 1
            pbs = [C.ps_b[b0], C.ps_b[b0 + 1]]
            fns = [I("transpose", C.ps[:, b0 + k // 4, (k % 4) * 128:(k % 4 + 1) * 128], xs[:, k * 128:(k + 1) * 128], C.ident)
                   for k in range(8)]
            P.op("pe", fns, R=[xs_b, C.const_b], W=pbs)
            ot, ot_b = C.next_f32()
            if c % 2 == 0:
                P.op("act", I("activation", out=ot[:, :].rearrange("p (a b) -> p a b", a=2), in_=C.ps[:, b0:b0 + 2, :], func=AF.Copy), R=pbs, W=[ot_b])
            else:
                P.op("dve", I("tensor_copy", out=ot[:, :].rearrange("p (a b) -> p a b", a=2), in_=C.ps[:, b0:b0 + 2, :]), R=pbs, W=[ot_b])
            P.dma("sp", I("dma_start", out=out[t0:t0 + TH, c * 128:(c + 1) * 128].rearrange("(a p) n -> p a n", p=128),
                                                         in_=ot[:, :].rearrange("p (a n) -> p a n", a=8)), R=[ot_b], is_output=True)


_CACHE = {}


def _get_prog(key):
    if key not in _CACHE:
        _CACHE[key] = build_program(*key)
    return _CACHE[key]


NCORES = 8
NBATCH = 1


def _consts():
    c = np.zeros((128, 3, 128), np.float32)
    p = np.arange(128)[:, None]
    t = np.arange(128)[None, :]
    c[:, 0, :] = (p == t)
    c[:, 1, :] = (p <= t)
    c[:, 2, :] = (p > t)
    return c


def kernel(x, mix_norm, w_in, conv_w, gla_a_up, gla_a_bias, gla_norm, w_out, ffn_norm, w_gate, w_up, w_down, final_norm):
    x = np.asarray(x, dtype=np.float32)
    B = x.shape[0]
    assert B == NCORES * NBATCH
    nc = _get_prog((tuple(range(DEPTH)), True, True, DEPTH, NBATCH))
    shared = {
        "mix_norm": np.asarray(mix_norm, np.float32), "w_in": np.asarray(w_in, np.float32),
        "conv_w": np.asarray(conv_w, np.float32), "gla_a_up": np.asarray(gla_a_up, np.float32),
        "gla_a_bias": np.asarray(gla_a_bias, np.float32).reshape(DEPTH, 1, KEYW),
        "gla_norm": np.asarray(gla_norm, np.float32).reshape(DEPTH, 1, GLAW),
        "w_out": np.asarray(w_out, np.float32), "ffn_norm": np.asarray(ffn_norm, np.float32),
        "w_gate": np.asarray(w_gate, np.float32), "w_up": np.asarray(w_up, np.float32),
        "w_down": np.asarray(w_down, np.float32), "final_norm": np.asarray(final_norm, np.float32).reshape(1, D),
        "consts": _consts(),
    }
    in_maps = [dict(shared, x=x[c * NBATCH:(c + 1) * NBATCH]) for c in range(NCORES)]
    res = run_bass_kernel_spmd(nc, in_maps, core_ids=list(range(NCORES)))
    return np.concatenate([np.asarray(r["out"], dtype=np.float32) for r in res.results], axis=0)
```

```python
import os
import numpy as np
from contextlib import ExitStack
import concourse.bass as bass
import concourse.mybir as mybir
from concourse.bass_utils import run_bass_kernel_spmd

F32 = mybir.dt.float32
BF16 = mybir.dt.bfloat16
AF = mybir.ActivationFunctionType
ALU = mybir.AluOpType

D = 4096
S = 2048
DEPTH = 4
CONVW = 2048
KEYW = 1024
GLAW = 2048
HEADS = 4
DK = 256
DV = 512
RANK = 16
DFF = 11008
INCOLS = 12304
EPS = 1e-6
KC = D // 128
TH = 1024
NHALF = S // TH
O_CB, O_CC, O_CH, O_Q, O_K, O_V, O_G, O_A = 0, 2048, 4096, 6144, 7168, 8192, 10240, 12288


class Buf:
    __slots__ = ("w", "r", "name")

    def __init__(self, name=""):
        self.w = None
        self.r = {}
        self.name = name


class Sem:
    __slots__ = ("h", "n")

    def __init__(self, h):
        self.h = h
        self.n = 0


class Prog:
    ENG = ("sp", "act", "dve", "pool", "pe")
    NSLOT = 8

    def __init__(self, nc, es):
        self.nc = nc
        self.q = {e: [] for e in self.ENG}
        self.esem = {e: Sem(es.enter_context(nc.semaphore("e_" + e))) for e in self.ENG}
        self.dsem = {e: [Sem(es.enter_context(nc.semaphore(f"d_{e}{i}"))) for i in range(self.NSLOT)]
                     for e in ("sp", "pool", "act")}
        self.dcnt = {e: 0 for e in ("sp", "pool", "act")}
        self.out_toks = []

    def _waits(self, eng, R, W):
        ws = {}

        def add(tok):
            if tok is None:
                return
            s, v = tok
            if ws.get(s, (None, -1))[1] < v:
                ws[s] = (s, v)
        for b in R:
            add(b.w)
        for b in W:
            add(b.w)
            for s, v in b.r.items():
                add((s, v))
        if eng == "pe":
            ws.pop(self.esem["pe"], None)
        return list(ws.values())

    def _commit(self, tok, R, W):
        s, v = tok
        for b in R:
            if b.r.get(s, -1) < v:
                b.r[s] = v
        for b in W:
            b.w = tok
            b.r = {}

    def op(self, eng, fns, R=(), W=()):
        if not isinstance(fns, (list, tuple)):
            fns = [fns]
        waits = self._waits(eng, R, W)
        sem = self.esem[eng]
        sem.n += 1
        tok = (sem, sem.n)
        self.q[eng].append((list(fns), waits, sem, 1))
        self._commit(tok, R, W)
        return tok

    def dma(self, eng, fn, R=(), W=(), is_output=False):
        waits = self._waits(eng, R, W)
        i = self.dcnt[eng]
        self.dcnt[eng] += 1
        sem = self.dsem[eng][i % self.NSLOT]
        if sem.n > 0:
            waits.append((sem, sem.n))
        sem.n += 16
        tok = (sem, sem.n)
        self.q[eng].append(([fn], waits, sem, 16))
        self._commit(tok, R, W)
        if is_output:
            self.out_toks.append(tok)
        return tok

    def finish(self):
        waits = [(s, s.n) for s in self.esem.values() if s.n > 0]
        for e in self.dsem:
            waits += [(s, s.n) for s in self.dsem[e] if s.n > 0]
        self.q["sp"].append(([I("nop", )], waits, None, 0))

    def emit(self):
        nc = self.nc
        engs = {"sp": "sync", "act": "scalar", "dve": "vector", "pool": "gpsimd", "pe": "tensor"}

        def run(name, e):
            seen = {}
            for fns, waits, sem, inc in self.q[name]:
                for s, v in waits:
                    if seen.get(s, -1) >= v:
                        continue
                    seen[s] = v
                    e.wait_ge(s.h, v)
                ins = None
                for f in fns:
                    ins = f(e)
                if sem is not None:
                    ins.then_inc(sem.h, inc)
        with nc.Block() as block:
            for name in self.ENG:
                getattr(block, engs[name])(lambda e, name=name: run(name, e))


class Ctx:
    pass


def I(method, *a, **k):
    return lambda e: getattr(e, method)(*a, **k)


def build_program(layers, first, last, nlayers_in, NB=1):
    nc = bass.Bass("TRN2", target_bir_lowering=False)
    L = nlayers_in
    dt = nc.dram_tensor
    if first:
        x_in = dt("x", [NB, S, D], F32, kind="ExternalInput")
    else:
        x_in = dt("xT_in", [KC, 128, S], F32, kind="ExternalInput")
    mix_norm = dt("mix_norm", [L, D], F32, kind="ExternalInput")
    w_in = dt("w_in", [L, D, INCOLS], F32, kind="ExternalInput")
    conv_w = dt("conv_w", [L, 3, CONVW], F32, kind="ExternalInput")
    a_up = dt("gla_a_up", [L, RANK, KEYW], F32, kind="ExternalInput")
    a_bias = dt("gla_a_bias", [L, 1, KEYW], F32, kind="ExternalInput")
    gla_norm = dt("gla_norm", [L, 1, GLAW], F32, kind="ExternalInput")
    w_out = dt("w_out", [L, D, D], F32, kind="ExternalInput")
    ffn_norm = dt("ffn_norm", [L, D], F32, kind="ExternalInput")
    w_gate = dt("w_gate", [L, D, DFF], F32, kind="ExternalInput")
    w_up = dt("w_up", [L, D, DFF], F32, kind="ExternalInput")
    w_down = dt("w_down", [L, DFF, D], F32, kind="ExternalInput")
    final_norm = dt("final_norm", [1, D], F32, kind="ExternalInput")
    consts = dt("consts", [128, 3, 128], F32, kind="ExternalInput")
    if last:
        out = dt("out", [NB, S, D], F32, kind="ExternalOutput")
    else:
        out = dt("xT_out", [KC, 128, S], F32, kind="ExternalOutput")
    kd = "ExternalOutput" if os.environ.get("KDEBUG") else "Internal"
    xT = dt("xT_s", [KC, 128, S], F32, kind=kd)
    PT = dt("PT_s", [96, 128, S], BF16, kind=kd)
    AL = dt("AL_s", [RANK, S], F32, kind=kd)
    KVtm = dt("KV_s", [S, KEYW + GLAW], BF16, kind=kd)
    YT = dt("YT_s", [KC, 128, S], BF16, kind=kd)
    HT = dt("HT_s", [DFF // 128, 128, S], BF16, kind=kd)

    with ExitStack() as es:
        P = Prog(nc, es)
        sb = lambda name, shape, dtype: es.enter_context(nc.sbuf_tensor(name, shape, dtype))
        C = Ctx()
        C.res = sb("res", [128, KC, TH], BF16)
        C.res_b = [Buf(f"res{c}") for c in range(KC)]
        NWB = 2
        C.wb = [sb(f"wb{i}", [128, KC, 256], BF16) for i in range(NWB)]
        C.wb_b = [[Buf(), Buf()] for _ in range(NWB)]
        C.wcnt = 0
        NF = 5
        C.f32 = [sb(f"f32_{i}", [128, TH], F32) for i in range(NF)]
        C.f32_b = [Buf() for _ in range(NF)]
        C.fcnt = 0
        C.rs = sb("rs", [128, TH], F32)
        C.rs_b = Buf()
        NB16 = 6
        C.b16 = [sb(f"b16_{i}", [128, TH], BF16) for i in range(NB16)]
        C.b16_b = [Buf() for _ in range(NB16)]
        C.bcnt = 0
        C.ps = es.enter_context(nc.psum_tensor("ps", [128, 8, 512], F32))
        C.ps_b = [Buf(f"ps{i}") for i in range(8)]
        C.pcnt = 0
        C.ones_bf = sb("ones_bf", [128, 128], BF16)
        C.epsc = sb("epsc", [128, 1], F32)
        C.gains = sb("gains", [128, 3, KC], F32)
        C.gains_b = Buf()
        C.convw = sb("convw", [128, 3, 16], F32)
        C.convw_b = Buf()
        C.const_b = Buf()

        def next_f32():
            i = C.fcnt % NF
            C.fcnt += 1
            return C.f32[i], C.f32_b[i]

        def next_b16():
            i = C.bcnt % NB16
            C.bcnt += 1
            return C.b16[i], C.b16_b[i]

        C.next_f32 = next_f32
        C.next_b16 = next_b16

        C.cst = sb("cst", [128, 3, 128], F32)
        C.ident = C.cst[:, 0, :]
        C.tri = C.cst[:, 1, :]
        C.su = C.cst[:, 2, :]
        C.onec = sb("onec", [128, 1], F32)
        P.dma("sp", I("dma_start", out=C.cst[:, :, :], in_=consts[:, :, :]), W=[C.const_b])

        def setup(e):
            e.memset(C.ones_bf[:, :], 1.0)
            e.memset(C.onec[:, :], 1.0)
            return e.memset(C.epsc[:, :], EPS)
        P.op("pool", setup, W=[C.const_b])
        xT_b = [Buf(f"xT{c}") for c in range(KC)]
        PT_b = [Buf(f"PT{c}") for c in range(96)]
        AL_b = Buf("AL")
        KV_b = [Buf(f"KV{i}") for i in range(12)]
        YT_b = [Buf(f"YT{c}") for c in range(KC)]
        HT_b = [Buf(f"HT{c}") for c in range(DFF // 128)]
        C.xT, C.xT_b, C.PT, C.PT_b, C.AL, C.AL_b = xT, xT_b, PT, PT_b, AL, AL_b
        C.KV, C.KV_b, C.YT, C.YT_b, C.HT, C.HT_b = KVtm, KV_b, YT, YT_b, HT, HT_b

        for b in range(NB):
            if first:
                stage_transpose_in(P, C, x_in[b])
            else:
                for c in range(KC):
                    P.dma("sp", I("dma_start", out=xT[c, :, :], in_=x_in[c, :, :]), W=[xT_b[c]])
            for li in layers:
                stage_load_gains(P, C, mix_norm, ffn_norm, final_norm, conv_w, li)
                stage_inproj(P, C, w_in[li], mix_norm_idx=0)
                side = conv_iter(P, C)
                stage_gla(P, C, a_up[li], a_bias[li], gla_norm[li], es, sb, side=side)
                for _ in side:
                    pass
                stage_gemm_resid(P, C, None, w_out[li], [(0, KC)], src="YT")
                stage_gateup(P, C, w_gate[li], w_up[li])
                stage_gemm_resid(P, C, None, w_down[li], [(0, 29), (29, 29), (58, 28)], src="HT")
            if last:
                stage_final(P, C, out[b])
            else:
                for c in range(KC):
                    P.dma("sp", I("dma_start", out=out[c, :, :], in_=xT[c, :, :]), R=[xT_b[c]], is_output=True)
        if os.environ.get("KSBUF"):
            print("SBUF remaining", nc.sbuf_bytes_remaining)
        P.finish()
        P.emit()
    return nc


def stage_transpose_in(P, C, x_in):
    for tt in range(S // 128):
        for g in range(KC // 8):
            xt, xt_b = C.next_f32()
            P.dma("sp", I("dma_start", out=xt[:, :], in_=x_in[tt * 128:(tt + 1) * 128, g * 1024:(g + 1) * 1024]),
                  W=[xt_b])
            b0 = (C.pcnt % 4) * 2
            C.pcnt += 1
            pbs = [C.ps_b[b0], C.ps_b[b0 + 1]]
            fns = []
            for k in range(8):
                fns.append(I("transpose",
                    C.ps[:, b0 + k // 4, (k % 4) * 128:(k % 4 + 1) * 128], xt[:, k * 128:(k + 1) * 128], C.ident))
            P.op("pe", fns, R=[xt_b, C.const_b], W=pbs)
            ot, ot_b = C.next_f32()
            eng = "act" if (tt * 4 + g) % 2 == 0 else "dve"
            if eng == "act":
                P.op("act", I("activation", out=ot[:, :].rearrange("p (a b) -> p a b", a=2), in_=C.ps[:, b0:b0 + 2, :], func=AF.Copy),
                     R=pbs, W=[ot_b])
            else:
                P.op("dve", I("tensor_copy", out=ot[:, :].rearrange("p (a b) -> p a b", a=2), in_=C.ps[:, b0:b0 + 2, :]),
                     R=pbs, W=[ot_b])
            P.dma("pool", I("dma_start",
                out=C.xT[g * 8:(g + 1) * 8, :, tt * 128:(tt + 1) * 128].rearrange("c p t -> p c t"),
                in_=ot[:, :].rearrange("p (c t) -> p c t", c=8)),
                R=[ot_b], W=[C.xT_b[g * 8 + k] for k in range(8)])


def stage_load_gains(P, C, mix_norm, ffn_norm, final_norm, conv_w, li):
    nc = P.nc
    srcs = [mix_norm[li:li + 1, :], ffn_norm[li:li + 1, :], final_norm[0:1, :]]
    for i, s in enumerate(srcs):
        P.dma("sp", I("dma_start", out=C.gains[:, i, :], in_=s.rearrange("o (c p) -> p (o c)", p=128),
                                                  allow_slow_non_contiguous=True), W=[C.gains_b])
    for k in range(3):
        P.dma("sp", I("dma_start", out=C.convw[:, k, :], in_=conv_w[li, k:k + 1, :].rearrange("o (c p) -> p (o c)", p=128),
                                             allow_slow_non_contiguous=True), W=[C.convw_b])


def norm_to_res(P, C, half, gidx):
    t0 = half * TH
    pbs = [C.ps_b[0], C.ps_b[1]]
    for c in range(KC):
        xs, xs_b = C.next_f32()
        P.dma("sp", I("dma_start", out=xs[:, :], in_=C.xT[c, :, t0:t0 + TH]), R=[C.xT_b[c]], W=[xs_b])
        sq, sq_b = C.next_b16()
        P.op("act", I("activation", out=sq[:, :], in_=xs[:, :], func=AF.Square), R=[xs_b], W=[sq_b])
        fns = [I("matmul", C.ps[:, t, :], C.ones_bf[:, :], sq[:, t * 512:(t + 1) * 512],
                                                  start=(c == 0), stop=(c == KC - 1)) for t in range(2)]
        P.op("pe", fns, R=[sq_b, C.const_b], W=pbs)
    rs, rs_b = C.rs, C.rs_b
    P.op("act", I("activation", out=rs[:, :].rearrange("p (a b) -> p a b", a=2), in_=C.ps[:, 0:2, :], func=AF.Sqrt,
                                              bias=C.epsc[:, 0:1], scale=1.0 / D), R=pbs + [C.const_b], W=[rs_b])
    P.op("dve", I("reciprocal", out=rs[:, :], in_=rs[:, :]), R=[rs_b], W=[rs_b])
    return rs, rs_b


def norm_apply(P, C, half, gidx, rs, rs_b):
    t0 = half * TH
    for c in range(KC):
        xs, xs_b = C.next_f32()
        P.dma("sp", I("dma_start", out=xs[:, :], in_=C.xT[c, :, t0:t0 + TH]), R=[C.xT_b[c]], W=[xs_b])
        eng = "dve"
        P.op(eng, I("scalar_tensor_tensor", out=C.res[:, c, :], in0=xs[:, :], scalar=C.gains[:, gidx, c:c + 1],
                                                              in1=rs[:, :], op0=ALU.mult, op1=ALU.mult),
             R=[xs_b, rs_b, C.gains_b], W=[C.res_b[c]])


def load_w(P, C, w_view, c0, kc, n0, ncols, sub=0):
    i = C.wcnt % len(C.wb)
    wb = C.wb[i]
    if ncols > 128:
        bufs = C.wb_b[i]
        C.wcnt += 1
        P.dma("pool", I("dma_start", out=wb[:, 0:kc, 0:ncols], in_=w_view[:, c0:c0 + kc, n0:n0 + ncols]), W=bufs)
    else:
        bufs = [C.wb_b[i][sub]]
        if sub == 1:
            C.wcnt += 1
        P.dma("pool", I("dma_start", out=wb[:, 0:kc, sub * 128:sub * 128 + ncols], in_=w_view[:, c0:c0 + kc, n0:n0 + ncols]), W=bufs)
    return wb, i


def mm_chunk(P, C, wb, wbuf, woff, m, kc, slot, first_k=True, last_k=True):
    b0 = slot * 2
    fns = []
    for c in range(kc):
        for t in range(2):
            fns.append(I("matmul", C.ps[0:m, b0 + t, :], wb[:, c, woff:woff + m], C.res[:, c, t * 512:(t + 1) * 512],
                                                    start=(first_k and c == 0), stop=(last_k and c == kc - 1)))
    pbs = [C.ps_b[b0], C.ps_b[b0 + 1]]
    if getattr(C, "res_fresh", False):
        C.res_fresh = False
        for c in range(kc):
            P.op("pe", fns[2 * c:2 * c + 2], R=[wbuf, C.res_b[c]], W=pbs)
    else:
        P.op("pe", fns, R=[wbuf] + C.res_b[0:kc], W=pbs)
    return b0, pbs


def stage_inproj(P, C, w_l, mix_norm_idx):
    w_view = w_l.rearrange("(c p) n -> p c n", p=128)
    fm_groups = [(n0, 256) for n0 in range(0, O_V, 256)] + [(n0, 256) for n0 in range(O_G, O_A, 256)]
    for half in range(NHALF):
        t0 = half * TH
        rs, rs_b = norm_to_res(P, C, half, mix_norm_idx)
        norm_apply(P, C, half, mix_norm_idx, rs, rs_b)
        C.res_fresh = True
        for (n0, ncols) in fm_groups:
            wb, wi = load_w(P, C, w_view, 0, KC, n0, ncols)
            for j in range(2):
                col = n0 + j * 128
                pt_idx = col // 128 if col < O_V else (col - O_G) // 128 + 80
                slot = C.pcnt % 4
                C.pcnt += 1
                b0, pbs = mm_chunk(P, C, wb, C.wb_b[wi][j], j * 128, 128, KC, slot)
                ot, ot_b = C.next_b16()
                evac_copy(P, C, b0, pbs, ot, ot_b, 128)
                P.dma("sp", I("dma_start", out=C.PT[pt_idx, :, t0:t0 + TH], in_=ot[:, :]),
                      R=[ot_b], W=[C.PT_b[pt_idx]])
        wb, wi = load_w(P, C, w_view, 0, KC, O_A, RANK, sub=0)
        C.wcnt += 1
        slot = C.pcnt % 4
        C.pcnt += 1
        b0, pbs = mm_chunk(P, C, wb, C.wb_b[wi][0], 0, RANK, KC, slot)
        ot, ot_b = C.next_f32()
        P.op("act", I("activation", out=ot[0:RANK, :].rearrange("p (a b) -> p a b", a=2), in_=C.ps[0:RANK, b0:b0 + 2, :], func=AF.Copy),
             R=pbs, W=[ot_b])
        P.dma("sp", I("dma_start", out=C.AL[:, t0:t0 + TH], in_=ot[0:RANK, :]), R=[ot_b], W=[C.AL_b])
        for gi, n0 in enumerate(range(O_K, O_G, 256)):
            wb, wi = load_w(P, C, w_view, 0, KC, n0, 256)
            st, st_b = C.next_b16()
            for tt in range(TH // 128):
                bank = (C.pcnt % 4) * 2 + (tt % 2)
                if tt % 2 == 1:
                    C.pcnt += 1
                fns = [I("matmul", C.ps[:, bank, 0:256], C.res[:, c, tt * 128:(tt + 1) * 128], wb[:, c, 0:256],
                                                                 start=(c == 0), stop=(c == KC - 1)) for c in range(KC)]
                P.op("pe", fns, R=C.wb_b[wi] + C.res_b, W=[C.ps_b[bank]])
                if tt % 4 == 0 and tt > 0:
                    st, st_b = C.next_b16()
                eng = "act" if tt % 2 == 0 else "dve"
                q4 = tt % 4
                if eng == "act":
                    P.op("act", I("activation", out=st[:, q4 * 256:(q4 + 1) * 256], in_=C.ps[:, bank, 0:256], func=AF.Copy),
                         R=[C.ps_b[bank]], W=[st_b])
                else:
                    P.op("dve", I("tensor_copy", out=st[:, q4 * 256:(q4 + 1) * 256], in_=C.ps[:, bank, 0:256]),
                         R=[C.ps_b[bank]], W=[st_b])
                if q4 == 3:
                    tbase = t0 + (tt - 3) * 128
                    P.dma("sp", I("dma_start",
                        out=C.KV[tbase:tbase + 512, gi * 256:(gi + 1) * 256].rearrange("(a p) n -> p a n", p=128),
                        in_=st[:, :].rearrange("p (a n) -> p a n", a=4)), R=[st_b], W=[C.KV_b[gi]])


def evac_copy(P, C, b0, pbs, ot, ot_b, m):
    C.ecnt = getattr(C, "ecnt", 0) + 1
    o3 = ot[0:m, :].rearrange("p (a b) -> p a b", a=2)
    if C.ecnt % 2 == 0:
        P.op("act", I("activation", out=o3, in_=C.ps[0:m, b0:b0 + 2, :], func=AF.Copy), R=pbs, W=[ot_b])
    else:
        P.op("dve", I("tensor_copy", out=o3, in_=C.ps[0:m, b0:b0 + 2, :]), R=pbs, W=[ot_b])


def conv_iter(P, C):
    for j in range(CONVW // 128):
        for half in range(NHALF):
            t0 = half * TH
            cb, cb_b = C.next_b16()
            cc, cc_b = C.next_b16()
            ch, ch_b = C.next_b16()
            P.dma("sp", I("dma_start", out=cb[:, :], in_=C.PT[j, :, t0:t0 + TH]), R=[C.PT_b[j]], W=[cb_b])
            u, u_b = C.next_f32()
            u2, u2_b = C.next_f32()
            if half == 0:
                P.op("pool", [I("memset", cc[:, 0:2], 0.0), I("memset", ch[:, 0:2], 0.0)], W=[cc_b, ch_b])
                P.dma("sp", I("dma_start", out=cc[:, 2:TH], in_=C.PT[16 + j, :, 0:TH - 2]), R=[C.PT_b[16 + j]], W=[cc_b])
                P.dma("sp", I("dma_start", out=ch[:, 2:TH], in_=C.PT[32 + j, :, 0:TH - 2]), R=[C.PT_b[32 + j]], W=[ch_b])
            else:
                P.dma("sp", I("dma_start", out=cc[:, :], in_=C.PT[16 + j, :, t0 - 2:t0 + TH - 2]), R=[C.PT_b[16 + j]], W=[cc_b])
                P.dma("sp", I("dma_start", out=ch[:, :], in_=C.PT[32 + j, :, t0 - 2:t0 + TH - 2]), R=[C.PT_b[32 + j]], W=[ch_b])
            c2, c2_b = C.next_b16()
            P.dma("sp", I("dma_start", out=c2[:, 0:2], in_=C.PT[16 + j, :, t0 + TH - 2:t0 + TH]), R=[C.PT_b[16 + j]], W=[c2_b])
            P.dma("sp", I("dma_start", out=c2[:, 2:4], in_=C.PT[32 + j, :, t0 + TH - 2:t0 + TH]), R=[C.PT_b[32 + j]], W=[c2_b])
            P.op("pool", I("tensor_tensor", out=u[:, :], in0=cc[:, :], in1=ch[:, :], op=ALU.mult), R=[cc_b, ch_b], W=[u_b])
            P.op("dve", I("tensor_tensor", out=u2[:, 0:2], in0=c2[:, 0:2], in1=c2[:, 2:4], op=ALU.mult), R=[c2_b], W=[u2_b])
            y, y_b = C.next_f32()
            w0 = C.convw[:, 0, j:j + 1]
            w1 = C.convw[:, 1, j:j + 1]
            w2 = C.convw[:, 2, j:j + 1]
            P.op("act", I("activation", out=y[:, :], in_=u[:, :], func=AF.Copy, scale=w0), R=[u_b, C.convw_b], W=[y_b])
            P.op("dve", I("scalar_tensor_tensor", out=y[:, 0:TH - 1], in0=u[:, 1:TH], scalar=w1, in1=y[:, 0:TH - 1], op0=ALU.mult, op1=ALU.add),
                 R=[u_b, C.convw_b, y_b], W=[y_b])
            P.op("dve", I("scalar_tensor_tensor", out=y[:, TH - 1:TH], in0=u2[:, 0:1], scalar=w1, in1=y[:, TH - 1:TH], op0=ALU.mult, op1=ALU.add),
                 R=[u2_b, C.convw_b, y_b], W=[y_b])
            P.op("dve", I("scalar_tensor_tensor", out=y[:, 0:TH - 2], in0=u[:, 2:TH], scalar=w2, in1=y[:, 0:TH - 2], op0=ALU.mult, op1=ALU.add),
                 R=[u_b, C.convw_b, y_b], W=[y_b])
            P.op("dve", I("scalar_tensor_tensor", out=y[:, TH - 2:TH], in0=u2[:, 0:2], scalar=w2, in1=y[:, TH - 2:TH], op0=ALU.mult, op1=ALU.add),
                 R=[u2_b, C.convw_b, y_b], W=[y_b])
            yb, yb_b = C.next_b16()
            P.op("pool", I("tensor_tensor", out=yb[:, :], in0=y[:, :], in1=cb[:, :], op=ALU.mult), R=[y_b, cb_b], W=[yb_b])
            P.dma("sp", I("dma_start", out=C.YT[j, :, t0:t0 + TH], in_=yb[:, :]), R=[yb_b], W=[C.YT_b[j]])
            yield


def stage_gla(P, C, a_up_l, a_bias_l, gnorm_l, es, sb, side=None):
    nc = P.nc
    G = C.gla = getattr(C, "gla", None) or Ctx()
    if not hasattr(G, "init"):
        G.init = True
        def view(ap, a):
            return ap.rearrange("p c t -> p (c t)").rearrange("p (a s) -> p a s", a=a)
        G.qT = view(C.res[:, 0:4, :], 2); G.qT_bs = C.res_b[0:4]
        G.kT = view(C.res[:, 4:8, :], 2); G.kT_bs = C.res_b[4:8]
        G.gT = view(C.res[:, 8:16, :], 4); G.gT_bs = C.res_b[8:16]
        G.vtm = view(C.res[:, 16:24, :], S // 128); G.vtm_bs = C.res_b[16:24]
        G.ktm = view(C.res[:, 24:28, :], S // 128); G.ktm_bs = C.res_b[24:28]
        G.alT = sb("g_alT", [RANK + 1, S], F32); G.alT_b = Buf()
        G.aup = sb("g_aup", [RANK + 1, KEYW], F32); G.aup_b = Buf()
        G.gn = sb("g_gn", [128, 16], F32); G.gn_b = Buf()
        G.state = sb("g_state", [128, 2, DV], F32); G.state_b = Buf()
        G.state16 = sb("g_state16", [128, 2, DV], BF16); G.state16_b = Buf()
        NT = 3
        G.nls = [sb(f"g_nls{i}", [128, DK], F32) for i in range(NT)]; G.nls_b = [Buf() for _ in range(NT)]
        G.e1 = [sb(f"g_e1{i}", [128, 2, 128], F32) for i in range(NT)]; G.e1_b = [Buf() for _ in range(NT)]
        G.e2 = [sb(f"g_e2{i}", [128, 2, 128], F32) for i in range(NT)]; G.e2_b = [Buf() for _ in range(NT)]
        G.etot = [sb(f"g_etot{i}", [128, 2], F32) for i in range(NT)]; G.etot_b = [Buf() for _ in range(NT)]
        G.qd = [sb(f"g_qd{i}", [128, 2, 128], BF16) for i in range(NT)]; G.qd_b = [Buf() for _ in range(NT)]
        G.ki = [sb(f"g_ki{i}", [128, 2, 128], BF16) for i in range(NT)]; G.ki_b = [Buf() for _ in range(NT)]
        G.kd = [sb(f"g_kd{i}", [128, DK], BF16) for i in range(NT)]; G.kd_b = [Buf() for _ in range(NT)]
        G.esuf = [sb(f"g_esuf{i}", [128, DK], F32) for i in range(NT)]; G.esuf_b = [Buf() for _ in range(NT)]
        G.sT = [sb(f"g_sT{i}", [128, 128], BF16) for i in range(NT)]; G.sT_b = [Buf() for _ in range(NT)]
        G.o = [sb(f"g_o{i}", [128, 4, 128], F32) for i in range(NT)]; G.o_b = [Buf() for _ in range(NT)]
        G.sq = [sb(f"g_sq{i}", [128, 4, 128], BF16) for i in range(NT)]; G.sq_b = [Buf() for _ in range(NT)]
        G.rstd = [sb(f"g_rstd{i}", [128, 128], F32) for i in range(NT)]; G.rstd_b = [Buf() for _ in range(NT)]
        G.sg = [sb(f"g_sg{i}", [128, 4, 128], F32) for i in range(NT)]; G.sg_b = [Buf() for _ in range(NT)]
        G.y = view(C.wb[0][:, :, :], 4); G.y_bs = C.wb_b[0]
        G.NT = NT
        G.cnt = 0
    NT = G.NT
    P.dma("sp", I("dma_start", out=G.aup[0:RANK, :], in_=a_up_l[:, :]), W=[G.aup_b])
    P.dma("sp", I("dma_start", out=G.aup[RANK:RANK + 1, :], in_=a_bias_l[:, :]), W=[G.aup_b])
    P.dma("sp", I("dma_start", out=G.gn[:, :], in_=gnorm_l.rearrange("o (c p) -> p (o c)", p=128), allow_slow_non_contiguous=True), W=[G.gn_b])
    P.op("pool", I("memset", G.alT[:, :], 1.0), W=[G.alT_b])
    P.dma("sp", I("dma_start", out=G.alT[0:RANK, :], in_=C.AL[:, :]), R=[C.AL_b], W=[G.alT_b])
    for h in range(HEADS):
        for c in range(2):
            P.dma("sp", I("dma_start", out=G.qT[:, c, :], in_=C.PT[48 + h * 2 + c, :, :]), R=[C.PT_b[48 + h * 2 + c]], W=G.qT_bs)
            P.dma("sp", I("dma_start", out=G.kT[:, c, :], in_=C.PT[56 + h * 2 + c, :, :]), R=[C.PT_b[56 + h * 2 + c]], W=G.kT_bs)
        for c in range(4):
            P.dma("sp", I("dma_start", out=G.gT[:, c, :], in_=C.PT[80 + h * 4 + c, :, :]), R=[C.PT_b[80 + h * 4 + c]], W=G.gT_bs)
        P.dma("sp", I("dma_start", out=G.ktm[:, :, :], in_=C.KV[:, h * DK:(h + 1) * DK].rearrange("(a p) n -> p a n", p=128)),
              R=C.KV_b, W=G.ktm_bs)
        P.dma("sp", I("dma_start", out=G.vtm[:, :, :], in_=C.KV[:, KEYW + h * DV:KEYW + (h + 1) * DV].rearrange("(a p) n -> p a n", p=128)),
              R=C.KV_b, W=G.vtm_bs)
        P.op("pool", [I("memset", G.state[:, :, :], 0.0), I("memset", G.state16[:, :, :], 0.0)], W=[G.state_b, G.state16_b])
        P.op("act", I("activation", out=G.gT[:, :, :], in_=G.gT[:, :, :], func=AF.Silu), R=G.gT_bs, W=G.gT_bs)

        def T(eng, fns, R, W):
            return lambda: P.op(eng, fns, R=R, W=W)

        def phaseA(tt, i):
            tsl = slice(tt * 128, (tt + 1) * 128)
            nls, nls_b = G.nls[i], G.nls_b[i]
            e1, e1_b, e2, e2_b, etot, etot_b = G.e1[i], G.e1_b[i], G.e2[i], G.e2_b[i], G.etot[i], G.etot_b[i]
            esuf, esuf_b = G.esuf[i], G.esuf_b[i]
            qd, qd_b, ki, ki_b, kd, kd_b = G.qd[i], G.qd_b[i], G.ki[i], G.ki_b[i], G.kd[i], G.kd_b[i]
            sT, sT_b = G.sT[i], G.sT_b[i]
            ncum = C.ps[:, 1, 0:256].rearrange("p (c t) -> p c t", c=2)
            st = []
            st.append(T("pe", I("matmul", C.ps[:, 0, 0:DK], G.alT[:, tsl], G.aup[:, h * DK:(h + 1) * DK], start=True, stop=True),
                        [G.alT_b, G.aup_b], [C.ps_b[0]]))
            st.append(T("act", I("activation", out=nls[:, :], in_=C.ps[:, 0, 0:DK], func=AF.Exp, scale=-1.0), [C.ps_b[0]], [nls_b]))
            st.append(T("act", I("activation", out=nls[:, :], in_=nls[:, :], func=AF.Ln, bias=C.onec[:, 0:1], scale=1.0), [nls_b, C.const_b], [nls_b]))
            st.append(T("pe", [I("matmul", C.ps[:, 1, c * 128:(c + 1) * 128], nls[:, c * 128:(c + 1) * 128], C.tri, start=True, stop=True) for c in range(2)],
                        [nls_b, C.const_b], [C.ps_b[1]]))
            st.append(T("pe", I("matmul", C.ps[:, 2, 0:DK], C.su, nls[:, :], start=True, stop=True), [nls_b, C.const_b], [C.ps_b[2]]))
            st.append(T("act", I("activation", out=e1[:, :, :], in_=ncum, func=AF.Exp, scale=-1.0 / 16.0), [C.ps_b[1]], [e1_b]))
            st.append(T("act", I("activation", out=e2[:, :, :], in_=ncum, func=AF.Exp, scale=1.0 / 16.0), [C.ps_b[1]], [e2_b]))
            st.append(T("act", I("activation", out=etot[:, :], in_=ncum[:, :, 127], func=AF.Exp, scale=-1.0 / 16.0), [C.ps_b[1]], [etot_b]))
            st.append(T("act", I("activation", out=esuf[:, :], in_=C.ps[:, 2, 0:DK], func=AF.Exp, scale=-1.0 / 16.0), [C.ps_b[2]], [esuf_b]))
            st.append(T("dve", I("scalar_tensor_tensor", out=qd[:, :, :], in0=G.qT[:, :, tsl], scalar=float(DK) ** -0.5, in1=e1[:, :, :],
                                 op0=ALU.mult, op1=ALU.mult), G.qT_bs + [e1_b], [qd_b]))
            st.append(T("pool", I("tensor_tensor", out=ki[:, :, :], in0=G.kT[:, :, tsl], in1=e2[:, :, :], op=ALU.mult), G.kT_bs + [e2_b], [ki_b]))
            st.append(T("dve", I("tensor_tensor", out=kd[:, :], in0=G.ktm[:, tt, :], in1=esuf[:, :], op=ALU.mult), G.ktm_bs + [esuf_b], [kd_b]))
            st.append(T("pe", [I("matmul", C.ps[:, 3, 0:128], ki[:, c, :], qd[:, c, :], start=(c == 0), stop=(c == 1)) for c in range(2)],
                        [ki_b, qd_b], [C.ps_b[3]]))
            st.append(T("dve", I("tensor_tensor", out=sT[:, :], in0=C.ps[:, 3, 0:128], in1=C.tri, op=ALU.mult), [C.ps_b[3], C.const_b], [sT_b]))
            return st

        def phaseB(tt, i):
            tsl = slice(tt * 128, (tt + 1) * 128)
            etot, etot_b = G.etot[i], G.etot_b[i]
            qd, qd_b, kd, kd_b = G.qd[i], G.qd_b[i], G.kd[i], G.kd_b[i]
            sT, sT_b = G.sT[i], G.sT_b[i]
            o, o_b, sq, sq_b = G.o[i], G.o_b[i], G.sq[i], G.sq_b[i]
            rstd, rstd_b = G.rstd[i], G.rstd_b[i]
            ops4 = C.ps[:, 4, :].rearrange("p (c t) -> p c t", c=4)
            st = []
            fns = []
            for vc in range(4):
                fns.append(I("matmul", C.ps[:, 4, vc * 128:(vc + 1) * 128], G.vtm[:, tt, vc * 128:(vc + 1) * 128], sT[:, :], start=True, stop=False))
                for c in range(2):
                    fns.append(I("matmul", C.ps[:, 4, vc * 128:(vc + 1) * 128], G.state16[:, c, vc * 128:(vc + 1) * 128], qd[:, c, :],
                                 start=False, stop=(c == 1)))
            st.append(T("pe", fns, G.vtm_bs + [sT_b, G.state16_b, qd_b], [C.ps_b[4]]))
            st.append(T("pe", [I("matmul", C.ps[:, 5 + c, :], kd[:, c * 128:(c + 1) * 128], G.vtm[:, tt, :], start=True, stop=True) for c in range(2)],
                        [kd_b] + G.vtm_bs, [C.ps_b[5], C.ps_b[6]]))
            for c in range(2):
                st.append(T("dve", I("scalar_tensor_tensor", out=G.state[:, c, :], in0=G.state[:, c, :], scalar=etot[:, c:c + 1], in1=C.ps[:, 5 + c, :],
                                     op0=ALU.mult, op1=ALU.add), [etot_b, C.ps_b[5 + c], G.state_b], [G.state_b]))
            st.append(T("pool", I("tensor_copy", out=G.state16[:, :, :], in_=G.state[:, :, :]), [G.state_b], [G.state16_b]))
            st.append(T("act", I("activation", out=o[:, :, :], in_=ops4, func=AF.Copy), [C.ps_b[4]], [o_b]))
            st.append(T("act", I("activation", out=sq[:, :, :], in_=ops4, func=AF.Square), [C.ps_b[4]], [sq_b]))
            st.append(T("pe", [I("matmul", C.ps[:, 7, 0:128], C.ones_bf[:, :], sq[:, vc, :], start=(vc == 0), stop=(vc == 3)) for vc in range(4)],
                        [sq_b, C.const_b], [C.ps_b[7]]))
            st.append(T("act", I("activation", out=rstd[:, :], in_=C.ps[:, 7, 0:128], func=AF.Ln, bias=C.epsc[:, 0:1], scale=1.0 / DV),
                        [C.ps_b[7], C.const_b], [rstd_b]))
            st.append(T("act", I("activation", out=rstd[:, :], in_=rstd[:, :], func=AF.Exp, scale=-0.5), [rstd_b], [rstd_b]))
            for vc in range(4):
                st.append(T("dve", I("scalar_tensor_tensor", out=o[:, vc, :], in0=o[:, vc, :], scalar=G.gn[:, h * 4 + vc:h * 4 + vc + 1], in1=rstd[:, :],
                                     op0=ALU.mult, op1=ALU.mult), [o_b, rstd_b, G.gn_b], [o_b]))
            st.append(T("pool", I("tensor_tensor", out=G.y[:, :, tsl], in0=o[:, :, :], in1=G.gT[:, :, tsl], op=ALU.mult), [o_b] + G.gT_bs, G.y_bs))
            return st

        NTT = S // 128
        idx = [(G.cnt + k) % NT for k in range(NTT)]
        G.cnt += NTT
        for th in phaseA(0, idx[0]):
            th()
        for tt in range(NTT):
            a = phaseA(tt + 1, idx[tt + 1]) if tt + 1 < NTT else []
            b = phaseB(tt, idx[tt])
            for k in range(max(len(a), len(b))):
                if k < len(b):
                    b[k]()
                if k < len(a):
                    a[k]()
            if side is not None:
                next(side, None)
        for vc in range(4):
            P.dma("sp", I("dma_start", out=C.YT[16 + h * 4 + vc, :, :], in_=G.y[:, vc, :]), R=G.y_bs, W=[C.YT_b[16 + h * 4 + vc]])


def stage_gemm_resid(P, C, _unused, w_l, kpasses, src):
    w_view = w_l.rearrange("(c p) n -> p c n", p=128)
    srcT = C.YT if src == "YT" else C.HT
    src_b = C.YT_b if src == "YT" else C.HT_b
    for half in range(NHALF):
        t0 = half * TH
        for (c0, kc) in kpasses:
            for c in range(kc):
                P.dma("sp", I("dma_start", out=C.res[:, c, :], in_=srcT[c0 + c, :, t0:t0 + TH]), R=[src_b[c0 + c]], W=[C.res_b[c]])
            C.res_fresh = True
            for n0 in range(0, D, 256):
                wb, wi = load_w(P, C, w_view, c0, kc, n0, 256)
                for j in range(2):
                    nch = n0 // 128 + j
                    slot = C.pcnt % 4
                    C.pcnt += 1
                    b0, pbs = mm_chunk(P, C, wb, C.wb_b[wi][j], j * 128, 128, kc, slot)
                    xs, xs_b = C.next_f32()
                    P.dma("sp", I("dma_start", out=xs[:, :], in_=C.xT[nch, :, t0:t0 + TH]), R=[C.xT_b[nch]], W=[xs_b])
                    P.op("dve", I("tensor_tensor", out=xs[:, :].rearrange("p (a b) -> p a b", a=2), in0=xs[:, :].rearrange("p (a b) -> p a b", a=2),
                                                                         in1=C.ps[:, b0:b0 + 2, :], op=ALU.add), R=pbs + [xs_b], W=[xs_b])
                    P.dma("sp", I("dma_start", out=C.xT[nch, :, t0:t0 + TH], in_=xs[:, :]), R=[xs_b], W=[C.xT_b[nch]])


def stage_gateup(P, C, wg_l, wu_l):
    wg_view = wg_l.rearrange("(c p) n -> p c n", p=128)
    wu_view = wu_l.rearrange("(c p) n -> p c n", p=128)
    for half in range(NHALF):
        t0 = half * TH
        rs, rs_b = norm_to_res(P, C, half, 1)
        norm_apply(P, C, half, 1, rs, rs_b)
        C.res_fresh = True
        for j in range(DFF // 128):
            wb, wi = load_w(P, C, wg_view, 0, KC, j * 128, 128, sub=0)
            wb, wi = load_w(P, C, wu_view, 0, KC, j * 128, 128, sub=1)
            slot = C.pcnt % 4
            C.pcnt += 1
            bg, pg = mm_chunk(P, C, wb, C.wb_b[wi][0], 0, 128, KC, slot)
            slot = C.pcnt % 4
            C.pcnt += 1
            bu, pu = mm_chunk(P, C, wb, C.wb_b[wi][1], 128, 128, KC, slot)
            sg, sg_b = C.next_f32()
            P.op("act", I("activation", out=sg[:, :].rearrange("p (a b) -> p a b", a=2), in_=C.ps[:, bg:bg + 2, :], func=AF.Silu), R=pg, W=[sg_b])
            hb, hb_b = C.next_b16()
            P.op("dve", I("tensor_tensor", out=hb[:, :].rearrange("p (a b) -> p a b", a=2), in0=sg[:, :].rearrange("p (a b) -> p a b", a=2),
                                                                        in1=C.ps[:, bu:bu + 2, :], op=ALU.mult), R=pu + [sg_b], W=[hb_b])
            P.dma("sp", I("dma_start", out=C.HT[j, :, t0:t0 + TH], in_=hb[:, :]), R=[hb_b], W=[C.HT_b[j]])


def stage_final(P, C, out):
    for half in range(NHALF):
        t0 = half * TH
        rs, rs_b = norm_to_res(P, C, half, 2)
        for c in range(KC):
            xs, xs_b = C.next_f32()
            P.dma("sp", I("dma_start", out=xs[:, :], in_=C.xT[c, :, t0:t0 + TH]), R=[C.xT_b[c]], W=[xs_b])
            P.op("dve", I("scalar_tensor_tensor", out=xs[:, :], in0=xs[:, :], scalar=C.gains[:, 2, c:c + 1], in1=rs[:, :], op0=ALU.mult, op1=ALU.mult),
                 R=[xs_b, rs_b, C.gains_b], W=[xs_b])
            b0 = (C.pcnt % 4) * 2
            C.pcnt += 1
            pbs = [C.ps_b[b0], C.ps_b[b0 + 1]]
            fns = [I("transpose", C.ps[:, b0 + k // 4, (k % 4) * 128:(k % 4 + 1) * 128], xs[:, k * 128:(k + 1) * 128], C.ident)
                   for k in range(8)]
            P.op("pe", fns, R=[xs_b, C.const_b], W=pbs)
            ot, ot_b = C.next_f32()
            if c % 2 == 0:
                P.op("act", I("activation", out=ot[:, :].rearrange("p (a b) -> p a b", a=2), in_=C.ps[:, b0:b0 + 2, :], func=AF.Copy), R=pbs, W=[ot_b])
            else:
                P.op("dve", I("tensor_copy", out=ot[:, :].rearrange("p (a b) -> p a b", a=2), in_=C.ps[:, b0:b0 + 2, :]), R=pbs, W=[ot_b])
            P.dma("pool", I("dma_start", out=out[t0:t0 + TH, c * 128:(c + 1) * 128].rearrange("(a p) n -> p a n", p=128),
                                                         in_=ot[:, :].rearrange("p (a n) -> p a n", a=8)), R=[ot_b], is_output=True)


_CACHE = {}


def _get_prog(key):
    if key not in _CACHE:
        _CACHE[key] = build_program(*key)
    return _CACHE[key]


NCORES = 8
NBATCH = 1


def _consts():
    c = np.zeros((128, 3, 128), np.float32)
    p = np.arange(128)[:, None]
    t = np.arange(128)[None, :]
    c[:, 0, :] = (p == t)
    c[:, 1, :] = (p <= t)
    c[:, 2, :] = (p > t)
    return c


def kernel(x, mix_norm, w_in, conv_w, gla_a_up, gla_a_bias, gla_norm, w_out, ffn_norm, w_gate, w_up, w_down, final_norm):
    x = np.asarray(x, dtype=np.float32)
    B = x.shape[0]
    assert B == NCORES * NBATCH
    nc = _get_prog((tuple(range(DEPTH)), True, True, DEPTH, NBATCH))
    shared = {
        "mix_norm": np.asarray(mix_norm, np.float32), "w_in": np.asarray(w_in, np.float32),
        "conv_w": np.asarray(conv_w, np.float32), "gla_a_up": np.asarray(gla_a_up, np.float32),
        "gla_a_bias": np.asarray(gla_a_bias, np.float32).reshape(DEPTH, 1, KEYW),
        "gla_norm": np.asarray(gla_norm, np.float32).reshape(DEPTH, 1, GLAW),
        "w_out": np.asarray(w_out, np.float32), "ffn_norm": np.asarray(ffn_norm, np.float32),
        "w_gate": np.asarray(w_gate, np.float32), "w_up": np.asarray(w_up, np.float32),
        "w_down": np.asarray(w_down, np.float32), "final_norm": np.asarray(final_norm, np.float32).reshape(1, D),
        "consts": _consts(),
    }
    in_maps = [dict(shared, x=x[c * NBATCH:(c + 1) * NBATCH]) for c in range(NCORES)]
    res = run_bass_kernel_spmd(nc, in_maps, core_ids=list(range(NCORES)))
    return np.concatenate([np.asarray(r["out"], dtype=np.float32) for r in res.results], axis=0)
```

```python
import os
import numpy as np
from contextlib import ExitStack
import concourse.bass as bass
import concourse.mybir as mybir
from concourse.bass_utils import run_bass_kernel_spmd

F32 = mybir.dt.float32
BF16 = mybir.dt.bfloat16
AF = mybir.ActivationFunctionType
ALU = mybir.AluOpType

D = 4096
S = 2048
DEPTH = 4
CONVW = 2048
KEYW = 1024
GLAW = 2048
HEADS = 4
DK = 256
DV = 512
RANK = 16
DFF = 11008
INCOLS = 12304
EPS = 1e-6
KC = D // 128
TH = 1024
NHALF = S // TH
O_CB, O_CC, O_CH, O_Q, O_K, O_V, O_G, O_A = 0, 2048, 4096, 6144, 7168, 8192, 10240, 12288


class Buf:
    __slots__ = ("w", "r", "name")

    def __init__(self, name=""):
        self.w = None
        self.r = {}
        self.name = name


class Sem:
    __slots__ = ("h", "n")

    def __init__(self, h):
        self.h = h
        self.n = 0


class Prog:
    ENG = ("sp", "act", "dve", "pool", "pe")
    NSLOT = 8

    def __init__(self, nc, es):
        self.nc = nc
        self.q = {e: [] for e in self.ENG}
        self.esem = {e: Sem(es.enter_context(nc.semaphore("e_" + e))) for e in self.ENG}
        self.dsem = {e: [Sem(es.enter_context(nc.semaphore(f"d_{e}{i}"))) for i in range(self.NSLOT)]
                     for e in ("sp", "pool", "act")}
        self.dcnt = {e: 0 for e in ("sp", "pool", "act")}
        self.out_toks = []

    def _waits(self, eng, R, W):
        ws = {}

        def add(tok):
            if tok is None:
                return
            s, v = tok
            if ws.get(s, (None, -1))[1] < v:
                ws[s] = (s, v)
        for b in R:
            add(b.w)
        for b in W:
            add(b.w)
            for s, v in b.r.items():
                add((s, v))
        if eng == "pe":
            ws.pop(self.esem["pe"], None)
        return list(ws.values())

    def _commit(self, tok, R, W):
        s, v = tok
        for b in R:
            if b.r.get(s, -1) < v:
                b.r[s] = v
        for b in W:
            b.w = tok
            b.r = {}

    def op(self, eng, fns, R=(), W=()):
        if not isinstance(fns, (list, tuple)):
            fns = [fns]
        waits = self._waits(eng, R, W)
        sem = self.esem[eng]
        sem.n += 1
        tok = (sem, sem.n)
        self.q[eng].append((list(fns), waits, sem, 1))
        self._commit(tok, R, W)
        return tok

    def dma(self, eng, fn, R=(), W=(), is_output=False):
        waits = self._waits(eng, R, W)
        i = self.dcnt[eng]
        self.dcnt[eng] += 1
        sem = self.dsem[eng][i % self.NSLOT]
        if sem.n > 0:
            waits.append((sem, sem.n))
        sem.n += 16
        tok = (sem, sem.n)
        self.q[eng].append(([fn], waits, sem, 16))
        self._commit(tok, R, W)
        if is_output:
            self.out_toks.append(tok)
        return tok

    def finish(self):
        waits = [(s, s.n) for s in self.esem.values() if s.n > 0]
        for e in self.dsem:
            waits += [(s, s.n) for s in self.dsem[e] if s.n > 0]
        self.q["sp"].append(([I("nop", )], waits, None, 0))

    def emit(self):
        nc = self.nc
        engs = {"sp": "sync", "act": "scalar", "dve": "vector", "pool": "gpsimd", "pe": "tensor"}

        def run(name, e):
            seen = {}
            for fns, waits, sem, inc in self.q[name]:
                for s, v in waits:
                    if seen.get(s, -1) >= v:
                        continue
                    seen[s] = v
                    e.wait_ge(s.h, v)
                ins = None
                for f in fns:
                    ins = f(e)
                if sem is not None:
                    ins.then_inc(sem.h, inc)
        with nc.Block() as block:
            for name in self.ENG:
                getattr(block, engs[name])(lambda e, name=name: run(name, e))


class Ctx:
    pass


def I(method, *a, **k):
    return lambda e: getattr(e, method)(*a, **k)


def build_program(layers, first, last, nlayers_in, NB=1):
    nc = bass.Bass("TRN2", target_bir_lowering=False)
    L = nlayers_in
    dt = nc.dram_tensor
    if first:
        x_in = dt("x", [NB, S, D], F32, kind="ExternalInput")
    else:
        x_in = dt("xT_in", [KC, 128, S], F32, kind="ExternalInput")
    mix_norm = dt("mix_norm", [L, D], F32, kind="ExternalInput")
    w_in = dt("w_in", [L, D, INCOLS], F32, kind="ExternalInput")
    conv_w = dt("conv_w", [L, 3, CONVW], F32, kind="ExternalInput")
    a_up = dt("gla_a_up", [L, RANK, KEYW], F32, kind="ExternalInput")
    a_bias = dt("gla_a_bias", [L, 1, KEYW], F32, kind="ExternalInput")
    gla_norm = dt("gla_norm", [L, 1, GLAW], F32, kind="ExternalInput")
    w_out = dt("w_out", [L, D, D], F32, kind="ExternalInput")
    ffn_norm = dt("ffn_norm", [L, D], F32, kind="ExternalInput")
    w_gate = dt("w_gate", [L, D, DFF], F32, kind="ExternalInput")
    w_up = dt("w_up", [L, D, DFF], F32, kind="ExternalInput")
    w_down = dt("w_down", [L, DFF, D], F32, kind="ExternalInput")
    final_norm = dt("final_norm", [1, D], F32, kind="ExternalInput")
    consts = dt("consts", [128, 3, 128], F32, kind="ExternalInput")
    if last:
        out = dt("out", [NB, S, D], F32, kind="ExternalOutput")
    else:
        out = dt("xT_out", [KC, 128, S], F32, kind="ExternalOutput")
    kd = "ExternalOutput" if os.environ.get("KDEBUG") else "Internal"
    xT = dt("xT_s", [KC, 128, S], F32, kind=kd)
    PT = dt("PT_s", [96, 128, S], BF16, kind=kd)
    AL = dt("AL_s", [RANK, S], F32, kind=kd)
    KVtm = dt("KV_s", [S, KEYW + GLAW], BF16, kind=kd)
    YT = dt("YT_s", [KC, 128, S], BF16, kind=kd)
    HT = dt("HT_s", [DFF // 128, 128, S], BF16, kind=kd)

    with ExitStack() as es:
        P = Prog(nc, es)
        sb = lambda name, shape, dtype: es.enter_context(nc.sbuf_tensor(name, shape, dtype))
        C = Ctx()
        C.res = sb("res", [128, KC, TH], BF16)
        C.res_b = [Buf(f"res{c}") for c in range(KC)]
        NWB = 3
        C.wb = [sb(f"wb{i}", [128, KC, 256], BF16) for i in range(NWB)]
        C.wb_b = [[Buf(), Buf()] for _ in range(NWB)]
        C.wcnt = 0
        NF = 5
        C.f32 = [sb(f"f32_{i}", [128, TH], F32) for i in range(NF)]
        C.f32_b = [Buf() for _ in range(NF)]
        C.fcnt = 0
        C.rsh = [sb(f"rs{i}", [128, TH], F32) for i in range(NHALF)]
        C.rsh_b = [Buf() for _ in range(NHALF)]
        C.have_stats = {}
        NB16 = 6
        C.b16 = [sb(f"b16_{i}", [128, TH], BF16) for i in range(NB16)]
        C.b16_b = [Buf() for _ in range(NB16)]
        C.bcnt = 0
        C.ps = es.enter_context(nc.psum_tensor("ps", [128, 8, 512], F32))
        C.ps_b = [Buf(f"ps{i}") for i in range(8)]
        C.pcnt = 0
        C.ones_bf = sb("ones_bf", [128, 128], BF16)
        C.epsc = sb("epsc", [128, 1], F32)
        C.gains = sb("gains", [128, 3, KC], F32)
        C.gains_b = Buf()
        C.convw = sb("convw", [128, 3, 16], F32)
        C.convw_b = Buf()
        C.const_b = Buf()

        def next_f32():
            i = C.fcnt % NF
            C.fcnt += 1
            return C.f32[i], C.f32_b[i]

        def next_b16():
            i = C.bcnt % NB16
            C.bcnt += 1
            return C.b16[i], C.b16_b[i]

        C.next_f32 = next_f32
        C.next_b16 = next_b16

        C.cst = sb("cst", [128, 3, 128], F32)
        C.ident = C.cst[:, 0, :]
        C.tri = C.cst[:, 1, :]
        C.su = C.cst[:, 2, :]
        C.onec = sb("onec", [128, 1], F32)
        P.dma("sp", I("dma_start", out=C.cst[:, :, :], in_=consts[:, :, :]), W=[C.const_b])

        def setup(e):
            e.memset(C.ones_bf[:, :], 1.0)
            e.memset(C.onec[:, :], 1.0)
            return e.memset(C.epsc[:, :], EPS)
        P.op("pool", setup, W=[C.const_b])
        xT_b = [Buf(f"xT{c}") for c in range(KC)]
        PT_b = [Buf(f"PT{c}") for c in range(96)]
        AL_b = Buf("AL")
        KV_b = [Buf(f"KV{i}") for i in range(12)]
        YT_b = [Buf(f"YT{c}") for c in range(KC)]
        HT_b = [Buf(f"HT{c}") for c in range(DFF // 128)]
        C.xT, C.xT_b, C.PT, C.PT_b, C.AL, C.AL_b = xT, xT_b, PT, PT_b, AL, AL_b
        C.KV, C.KV_b, C.YT, C.YT_b, C.HT, C.HT_b = KVtm, KV_b, YT, YT_b, HT, HT_b

        for b in range(NB):
            if first:
                stage_transpose_in(P, C, x_in[b])
            else:
                for c in range(KC):
                    P.dma("sp", I("dma_start", out=xT[c, :, :], in_=x_in[c, :, :]), W=[xT_b[c]])
            for li in layers:
                stage_load_gains(P, C, mix_norm, ffn_norm, final_norm, conv_w, li)
                stage_inproj(P, C, w_in[li], mix_norm_idx=0)
                side = conv_iter(P, C)
                stage_gla(P, C, a_up[li], a_bias[li], gla_norm[li], es, sb, side=side)
                for _ in side:
                    pass
                stage_gemm_resid(P, C, None, w_out[li], [(0, KC)], src="YT")
                stage_gateup(P, C, w_gate[li], w_up[li])
                stage_gemm_resid(P, C, None, w_down[li], [(0, 29), (29, 29), (58, 28)], src="HT")
            if last:
                stage_final(P, C, out[b])
            else:
                for c in range(KC):
                    P.dma("sp", I("dma_start", out=out[c, :, :], in_=xT[c, :, :]), R=[xT_b[c]], is_output=True)
        if os.environ.get("KSBUF"):
            print("SBUF remaining", nc.sbuf_bytes_remaining)
        P.finish()
        P.emit()
    return nc


def stage_transpose_in(P, C, x_in):
    for tt in range(S // 128):
        for g in range(KC // 8):
            xt, xt_b = C.next_f32()
            P.dma("sp", I("dma_start", out=xt[:, :], in_=x_in[tt * 128:(tt + 1) * 128, g * 1024:(g + 1) * 1024]),
                  W=[xt_b])
            b0 = (C.pcnt % 4) * 2
            C.pcnt += 1
            pbs = [C.ps_b[b0], C.ps_b[b0 + 1]]
            fns = []
            for k in range(8):
                fns.append(I("transpose",
                    C.ps[:, b0 + k // 4, (k % 4) * 128:(k % 4 + 1) * 128], xt[:, k * 128:(k + 1) * 128], C.ident))
            P.op("pe", fns, R=[xt_b, C.const_b], W=pbs)
            ot, ot_b = C.next_f32()
            eng = "act" if (tt * 4 + g) % 2 == 0 else "dve"
            if eng == "act":
                P.op("act", I("activation", out=ot[:, :].rearrange("p (a b) -> p a b", a=2), in_=C.ps[:, b0:b0 + 2, :], func=AF.Copy),
                     R=pbs, W=[ot_b])
            else:
                P.op("dve", I("tensor_copy", out=ot[:, :].rearrange("p (a b) -> p a b", a=2), in_=C.ps[:, b0:b0 + 2, :]),
                     R=pbs, W=[ot_b])
            P.dma("pool", I("dma_start",
                out=C.xT[g * 8:(g + 1) * 8, :, tt * 128:(tt + 1) * 128].rearrange("c p t -> p c t"),
                in_=ot[:, :].rearrange("p (c t) -> p c t", c=8)),
                R=[ot_b], W=[C.xT_b[g * 8 + k] for k in range(8)])


def stage_load_gains(P, C, mix_norm, ffn_norm, final_norm, conv_w, li):
    nc = P.nc
    srcs = [mix_norm[li:li + 1, :], ffn_norm[li:li + 1, :], final_norm[0:1, :]]
    for i, s in enumerate(srcs):
        P.dma("sp", I("dma_start", out=C.gains[:, i, :], in_=s.rearrange("o (c p) -> p (o c)", p=128),
                                                  allow_slow_non_contiguous=True), W=[C.gains_b])
    for k in range(3):
        P.dma("sp", I("dma_start", out=C.convw[:, k, :], in_=conv_w[li, k:k + 1, :].rearrange("o (c p) -> p (o c)", p=128),
                                             allow_slow_non_contiguous=True), W=[C.convw_b])


def norm_to_res(P, C, half, gidx):
    t0 = half * TH
    if C.have_stats.get(half):
        C.have_stats[half] = False
        return C.rsh[half], C.rsh_b[half]
    pbs = [C.ps_b[0], C.ps_b[1]]
    for c in range(KC):
        xs, xs_b = C.next_f32()
        P.dma("sp", I("dma_start", out=xs[:, :], in_=C.xT[c, :, t0:t0 + TH]), R=[C.xT_b[c]], W=[xs_b])
        sq, sq_b = C.next_b16()
        P.op("act", I("activation", out=sq[:, :], in_=xs[:, :], func=AF.Square), R=[xs_b], W=[sq_b])
        fns = [I("matmul", C.ps[:, t, :], C.ones_bf[:, :], sq[:, t * 512:(t + 1) * 512],
                                                  start=(c == 0), stop=(c == KC - 1)) for t in range(2)]
        P.op("pe", fns, R=[sq_b, C.const_b], W=pbs)
    rs, rs_b = C.rsh[half], C.rsh_b[half]
    P.op("act", I("activation", out=rs[:, :].rearrange("p (a b) -> p a b", a=2), in_=C.ps[:, 0:2, :], func=AF.Sqrt,
                                              bias=C.epsc[:, 0:1], scale=1.0 / D), R=pbs + [C.const_b], W=[rs_b])
    P.op("dve", I("reciprocal", out=rs[:, :], in_=rs[:, :]), R=[rs_b], W=[rs_b])
    return rs, rs_b


def norm_apply(P, C, half, gidx, rs, rs_b):
    t0 = half * TH
    for c in range(KC):
        xs, xs_b = C.next_f32()
        P.dma("sp", I("dma_start", out=xs[:, :], in_=C.xT[c, :, t0:t0 + TH]), R=[C.xT_b[c]], W=[xs_b])
        eng = "dve"
        P.op(eng, I("scalar_tensor_tensor", out=C.res[:, c, :], in0=xs[:, :], scalar=C.gains[:, gidx, c:c + 1],
                                                              in1=rs[:, :], op0=ALU.mult, op1=ALU.mult),
             R=[xs_b, rs_b, C.gains_b], W=[C.res_b[c]])


def load_w(P, C, w_view, c0, kc, n0, ncols, sub=0):
    i = C.wcnt % len(C.wb)
    wb = C.wb[i]
    if ncols > 128:
        bufs = C.wb_b[i]
        C.wcnt += 1
        P.dma("pool", I("dma_start", out=wb[:, 0:kc, 0:ncols], in_=w_view[:, c0:c0 + kc, n0:n0 + ncols]), W=bufs)
    else:
        bufs = [C.wb_b[i][sub]]
        if sub == 1:
            C.wcnt += 1
        P.dma("pool", I("dma_start", out=wb[:, 0:kc, sub * 128:sub * 128 + ncols], in_=w_view[:, c0:c0 + kc, n0:n0 + ncols]), W=bufs)
    return wb, i


def mm_chunk(P, C, wb, wbuf, woff, m, kc, slot, first_k=True, last_k=True):
    b0 = slot * 2
    fns = []
    for c in range(kc):
        for t in range(2):
            fns.append(I("matmul", C.ps[0:m, b0 + t, :], wb[:, c, woff:woff + m], C.res[:, c, t * 512:(t + 1) * 512],
                                                    start=(first_k and c == 0), stop=(last_k and c == kc - 1)))
    pbs = [C.ps_b[b0], C.ps_b[b0 + 1]]
    if getattr(C, "res_fresh", False):
        C.res_fresh = False
        for c in range(kc):
            P.op("pe", fns[2 * c:2 * c + 2], R=[wbuf, C.res_b[c]], W=pbs)
    else:
        P.op("pe", fns, R=[wbuf] + C.res_b[0:kc], W=pbs)
    return b0, pbs


def stage_inproj(P, C, w_l, mix_norm_idx):
    w_view = w_l.rearrange("(c p) n -> p c n", p=128)
    fm_groups = [(n0, 256) for n0 in range(0, O_V, 256)] + [(n0, 256) for n0 in range(O_G, O_A, 256)]
    for half in range(NHALF):
        t0 = half * TH
        rs, rs_b = norm_to_res(P, C, half, mix_norm_idx)
        norm_apply(P, C, half, mix_norm_idx, rs, rs_b)
        C.res_fresh = True
        for (n0, ncols) in fm_groups:
            wb, wi = load_w(P, C, w_view, 0, KC, n0, ncols)
            for j in range(2):
                col = n0 + j * 128
                pt_idx = col // 128 if col < O_V else (col - O_G) // 128 + 80
                slot = C.pcnt % 4
                C.pcnt += 1
                b0, pbs = mm_chunk(P, C, wb, C.wb_b[wi][j], j * 128, 128, KC, slot)
                ot, ot_b = C.next_b16()
                evac_copy(P, C, b0, pbs, ot, ot_b, 128)
                P.dma("sp", I("dma_start", out=C.PT[pt_idx, :, t0:t0 + TH], in_=ot[:, :]),
                      R=[ot_b], W=[C.PT_b[pt_idx]])
        wb, wi = load_w(P, C, w_view, 0, KC, O_A, RANK, sub=0)
        C.wcnt += 1
        slot = C.pcnt % 4
        C.pcnt += 1
        b0, pbs = mm_chunk(P, C, wb, C.wb_b[wi][0], 0, RANK, KC, slot)
        ot, ot_b = C.next_f32()
        P.op("act", I("activation", out=ot[0:RANK, :].rearrange("p (a b) -> p a b", a=2), in_=C.ps[0:RANK, b0:b0 + 2, :], func=AF.Copy),
             R=pbs, W=[ot_b])
        P.dma("sp", I("dma_start", out=C.AL[:, t0:t0 + TH], in_=ot[0:RANK, :]), R=[ot_b], W=[C.AL_b])
        for gi, n0 in enumerate(range(O_K, O_G, 256)):
            wb, wi = load_w(P, C, w_view, 0, KC, n0, 256)
            st, st_b = C.next_b16()
            for tt in range(TH // 128):
                bank = (C.pcnt % 4) * 2 + (tt % 2)
                if tt % 2 == 1:
                    C.pcnt += 1
                fns = [I("matmul", C.ps[:, bank, 0:256], C.res[:, c, tt * 128:(tt + 1) * 128], wb[:, c, 0:256],
                                                                 start=(c == 0), stop=(c == KC - 1)) for c in range(KC)]
                P.op("pe", fns, R=C.wb_b[wi] + C.res_b, W=[C.ps_b[bank]])
                if tt % 4 == 0 and tt > 0:
                    st, st_b = C.next_b16()
                eng = "act" if tt % 2 == 0 else "dve"
                q4 = tt % 4
                if eng == "act":
                    P.op("act", I("activation", out=st[:, q4 * 256:(q4 + 1) * 256], in_=C.ps[:, bank, 0:256], func=AF.Copy),
                         R=[C.ps_b[bank]], W=[st_b])
                else:
                    P.op("dve", I("tensor_copy", out=st[:, q4 * 256:(q4 + 1) * 256], in_=C.ps[:, bank, 0:256]),
                         R=[C.ps_b[bank]], W=[st_b])
                if q4 == 3:
                    tbase = t0 + (tt - 3) * 128
                    P.dma("sp", I("dma_start",
                        out=C.KV[tbase:tbase + 512, gi * 256:(gi + 1) * 256].rearrange("(a p) n -> p a n", p=128),
                        in_=st[:, :].rearrange("p (a n) -> p a n", a=4)), R=[st_b], W=[C.KV_b[gi]])


def evac_copy(P, C, b0, pbs, ot, ot_b, m):
    C.ecnt = getattr(C, "ecnt", 0) + 1
    o3 = ot[0:m, :].rearrange("p (a b) -> p a b", a=2)
    if C.ecnt % 2 == 0:
        P.op("act", I("activation", out=o3, in_=C.ps[0:m, b0:b0 + 2, :], func=AF.Copy), R=pbs, W=[ot_b])
    else:
        P.op("dve", I("tensor_copy", out=o3, in_=C.ps[0:m, b0:b0 + 2, :]), R=pbs, W=[ot_b])


def conv_iter(P, C):
    for j in range(CONVW // 128):
        for half in range(NHALF):
            t0 = half * TH
            cb, cb_b = C.next_b16()
            cc, cc_b = C.next_b16()
            ch, ch_b = C.next_b16()
            P.dma("sp", I("dma_start", out=cb[:, :], in_=C.PT[j, :, t0:t0 + TH]), R=[C.PT_b[j]], W=[cb_b])
            u, u_b = C.next_f32()
            u2, u2_b = C.next_f32()
            if half == 0:
                P.op("pool", [I("memset", cc[:, 0:2], 0.0), I("memset", ch[:, 0:2], 0.0)], W=[cc_b, ch_b])
                P.dma("sp", I("dma_start", out=cc[:, 2:TH], in_=C.PT[16 + j, :, 0:TH - 2]), R=[C.PT_b[16 + j]], W=[cc_b])
                P.dma("sp", I("dma_start", out=ch[:, 2:TH], in_=C.PT[32 + j, :, 0:TH - 2]), R=[C.PT_b[32 + j]], W=[ch_b])
            else:
                P.dma("sp", I("dma_start", out=cc[:, :], in_=C.PT[16 + j, :, t0 - 2:t0 + TH - 2]), R=[C.PT_b[16 + j]], W=[cc_b])
                P.dma("sp", I("dma_start", out=ch[:, :], in_=C.PT[32 + j, :, t0 - 2:t0 + TH - 2]), R=[C.PT_b[32 + j]], W=[ch_b])
            c2, c2_b = C.next_b16()
            P.dma("sp", I("dma_start", out=c2[:, 0:2], in_=C.PT[16 + j, :, t0 + TH - 2:t0 + TH]), R=[C.PT_b[16 + j]], W=[c2_b])
            P.dma("sp", I("dma_start", out=c2[:, 2:4], in_=C.PT[32 + j, :, t0 + TH - 2:t0 + TH]), R=[C.PT_b[32 + j]], W=[c2_b])
            P.op("pool", I("tensor_tensor", out=u[:, :], in0=cc[:, :], in1=ch[:, :], op=ALU.mult), R=[cc_b, ch_b], W=[u_b])
            P.op("dve", I("tensor_tensor", out=u2[:, 0:2], in0=c2[:, 0:2], in1=c2[:, 2:4], op=ALU.mult), R=[c2_b], W=[u2_b])
            y, y_b = C.next_f32()
            w0 = C.convw[:, 0, j:j + 1]
            w1 = C.convw[:, 1, j:j + 1]
            w2 = C.convw[:, 2, j:j + 1]
            P.op("act", I("activation", out=y[:, :], in_=u[:, :], func=AF.Copy, scale=w0), R=[u_b, C.convw_b], W=[y_b])
            P.op("dve", I("scalar_tensor_tensor", out=y[:, 0:TH - 1], in0=u[:, 1:TH], scalar=w1, in1=y[:, 0:TH - 1], op0=ALU.mult, op1=ALU.add),
                 R=[u_b, C.convw_b, y_b], W=[y_b])
            P.op("dve", I("scalar_tensor_tensor", out=y[:, TH - 1:TH], in0=u2[:, 0:1], scalar=w1, in1=y[:, TH - 1:TH], op0=ALU.mult, op1=ALU.add),
                 R=[u2_b, C.convw_b, y_b], W=[y_b])
            P.op("dve", I("scalar_tensor_tensor", out=y[:, 0:TH - 2], in0=u[:, 2:TH], scalar=w2, in1=y[:, 0:TH - 2], op0=ALU.mult, op1=ALU.add),
                 R=[u_b, C.convw_b, y_b], W=[y_b])
            P.op("dve", I("scalar_tensor_tensor", out=y[:, TH - 2:TH], in0=u2[:, 0:2], scalar=w2, in1=y[:, TH - 2:TH], op0=ALU.mult, op1=ALU.add),
                 R=[u2_b, C.convw_b, y_b], W=[y_b])
            yb, yb_b = C.next_b16()
            P.op("pool", I("tensor_tensor", out=yb[:, :], in0=y[:, :], in1=cb[:, :], op=ALU.mult), R=[y_b, cb_b], W=[yb_b])
            P.dma("sp", I("dma_start", out=C.YT[j, :, t0:t0 + TH], in_=yb[:, :]), R=[yb_b], W=[C.YT_b[j]])
            yield


def stage_gla(P, C, a_up_l, a_bias_l, gnorm_l, es, sb, side=None):
    nc = P.nc
    G = C.gla = getattr(C, "gla", None) or Ctx()
    if not hasattr(G, "init"):
        G.init = True
        def view(ap, a):
            return ap.rearrange("p c t -> p (c t)").rearrange("p (a s) -> p a s", a=a)
        G.qT = view(C.res[:, 0:4, :], 2); G.qT_bs = C.res_b[0:4]
        G.kT = view(C.res[:, 4:8, :], 2); G.kT_bs = C.res_b[4:8]
        G.gT = view(C.res[:, 8:16, :], 4); G.gT_bs = C.res_b[8:16]
        G.vtm = view(C.res[:, 16:24, :], S // 128); G.vtm_bs = C.res_b[16:24]
        G.ktm = view(C.res[:, 24:28, :], S // 128); G.ktm_bs = C.res_b[24:28]
        G.alT = sb("g_alT", [RANK + 1, S], F32); G.alT_b = Buf()
        G.aup = sb("g_aup", [RANK + 1, KEYW], F32); G.aup_b = Buf()
        G.gn = sb("g_gn", [128, 16], F32); G.gn_b = Buf()
        G.state = sb("g_state", [128, 2, DV], F32); G.state_b = Buf()
        G.state16 = sb("g_state16", [128, 2, DV], BF16); G.state16_b = Buf()
        NT = 3
        G.nls = [sb(f"g_nls{i}", [128, DK], F32) for i in range(NT)]; G.nls_b = [Buf() for _ in range(NT)]
        G.e1 = [sb(f"g_e1{i}", [128, 2, 128], F32) for i in range(NT)]; G.e1_b = [Buf() for _ in range(NT)]
        G.e2 = [sb(f"g_e2{i}", [128, 2, 128], F32) for i in range(NT)]; G.e2_b = [Buf() for _ in range(NT)]
        G.etot = [sb(f"g_etot{i}", [128, 2], F32) for i in range(NT)]; G.etot_b = [Buf() for _ in range(NT)]
        G.qd = [sb(f"g_qd{i}", [128, 2, 128], BF16) for i in range(NT)]; G.qd_b = [Buf() for _ in range(NT)]
        G.ki = [sb(f"g_ki{i}", [128, 2, 128], BF16) for i in range(NT)]; G.ki_b = [Buf() for _ in range(NT)]
        G.kd = [sb(f"g_kd{i}", [128, DK], BF16) for i in range(NT)]; G.kd_b = [Buf() for _ in range(NT)]
        G.esuf = [sb(f"g_esuf{i}", [128, DK], F32) for i in range(NT)]; G.esuf_b = [Buf() for _ in range(NT)]
        G.sT = [sb(f"g_sT{i}", [128, 128], BF16) for i in range(NT)]; G.sT_b = [Buf() for _ in range(NT)]
        G.o = [sb(f"g_o{i}", [128, 4, 128], F32) for i in range(NT)]; G.o_b = [Buf() for _ in range(NT)]
        G.sq = [sb(f"g_sq{i}", [128, 4, 128], BF16) for i in range(NT)]; G.sq_b = [Buf() for _ in range(NT)]
        G.rstd = [sb(f"g_rstd{i}", [128, 128], F32) for i in range(NT)]; G.rstd_b = [Buf() for _ in range(NT)]
        G.sg = [sb(f"g_sg{i}", [128, 4, 128], F32) for i in range(NT)]; G.sg_b = [Buf() for _ in range(NT)]
        G.y = view(C.wb[0][:, :, :], 4); G.y_bs = C.wb_b[0]
        G.NT = NT
        G.cnt = 0
    NT = G.NT
    P.dma("sp", I("dma_start", out=G.aup[0:RANK, :], in_=a_up_l[:, :]), W=[G.aup_b])
    P.dma("sp", I("dma_start", out=G.aup[RANK:RANK + 1, :], in_=a_bias_l[:, :]), W=[G.aup_b])
    P.dma("sp", I("dma_start", out=G.gn[:, :], in_=gnorm_l.rearrange("o (c p) -> p (o c)", p=128), allow_slow_non_contiguous=True), W=[G.gn_b])
    P.op("pool", I("memset", G.alT[:, :], 1.0), W=[G.alT_b])
    P.dma("sp", I("dma_start", out=G.alT[0:RANK, :], in_=C.AL[:, :]), R=[C.AL_b], W=[G.alT_b])
    for h in range(HEADS):
        for c in range(2):
            P.dma("sp", I("dma_start", out=G.qT[:, c, :], in_=C.PT[48 + h * 2 + c, :, :]), R=[C.PT_b[48 + h * 2 + c]], W=G.qT_bs)
            P.dma("sp", I("dma_start", out=G.kT[:, c, :], in_=C.PT[56 + h * 2 + c, :, :]), R=[C.PT_b[56 + h * 2 + c]], W=G.kT_bs)
        for c in range(4):
            P.dma("sp", I("dma_start", out=G.gT[:, c, :], in_=C.PT[80 + h * 4 + c, :, :]), R=[C.PT_b[80 + h * 4 + c]], W=G.gT_bs)
        P.dma("sp", I("dma_start", out=G.ktm[:, :, :], in_=C.KV[:, h * DK:(h + 1) * DK].rearrange("(a p) n -> p a n", p=128)),
              R=C.KV_b, W=G.ktm_bs)
        P.dma("sp", I("dma_start", out=G.vtm[:, :, :], in_=C.KV[:, KEYW + h * DV:KEYW + (h + 1) * DV].rearrange("(a p) n -> p a n", p=128)),
              R=C.KV_b, W=G.vtm_bs)
        P.op("pool", [I("memset", G.state[:, :, :], 0.0), I("memset", G.state16[:, :, :], 0.0)], W=[G.state_b, G.state16_b])
        P.op("act", I("activation", out=G.gT[:, :, :], in_=G.gT[:, :, :], func=AF.Silu), R=G.gT_bs, W=G.gT_bs)

        def T(eng, fns, R, W):
            return lambda: P.op(eng, fns, R=R, W=W)

        def phaseA(tt, i):
            tsl = slice(tt * 128, (tt + 1) * 128)
            nls, nls_b = G.nls[i], G.nls_b[i]
            e1, e1_b, e2, e2_b, etot, etot_b = G.e1[i], G.e1_b[i], G.e2[i], G.e2_b[i], G.etot[i], G.etot_b[i]
            esuf, esuf_b = G.esuf[i], G.esuf_b[i]
            qd, qd_b, ki, ki_b, kd, kd_b = G.qd[i], G.qd_b[i], G.ki[i], G.ki_b[i], G.kd[i], G.kd_b[i]
            sT, sT_b = G.sT[i], G.sT_b[i]
            ncum = C.ps[:, 1, 0:256].rearrange("p (c t) -> p c t", c=2)
            st = []
            st.append(T("pe", I("matmul", C.ps[:, 0, 0:DK], G.alT[:, tsl], G.aup[:, h * DK:(h + 1) * DK], start=True, stop=True),
                        [G.alT_b, G.aup_b], [C.ps_b[0]]))
            st.append(T("act", I("activation", out=nls[:, :], in_=C.ps[:, 0, 0:DK], func=AF.Exp, scale=-1.0), [C.ps_b[0]], [nls_b]))
            st.append(T("act", I("activation", out=nls[:, :], in_=nls[:, :], func=AF.Ln, bias=C.onec[:, 0:1], scale=1.0), [nls_b, C.const_b], [nls_b]))
            st.append(T("pe", [I("matmul", C.ps[:, 1, c * 128:(c + 1) * 128], nls[:, c * 128:(c + 1) * 128], C.tri, start=True, stop=True) for c in range(2)],
                        [nls_b, C.const_b], [C.ps_b[1]]))
            st.append(T("pe", I("matmul", C.ps[:, 2, 0:DK], C.su, nls[:, :], start=True, stop=True), [nls_b, C.const_b], [C.ps_b[2]]))
            st.append(T("act", I("activation", out=e1[:, :, :], in_=ncum, func=AF.Exp, scale=-1.0 / 16.0), [C.ps_b[1]], [e1_b]))
            st.append(T("act", I("activation", out=e2[:, :, :], in_=ncum, func=AF.Exp, scale=1.0 / 16.0), [C.ps_b[1]], [e2_b]))
            st.append(T("act", I("activation", out=etot[:, :], in_=ncum[:, :, 127], func=AF.Exp, scale=-1.0 / 16.0), [C.ps_b[1]], [etot_b]))
            st.append(T("act", I("activation", out=esuf[:, :], in_=C.ps[:, 2, 0:DK], func=AF.Exp, scale=-1.0 / 16.0), [C.ps_b[2]], [esuf_b]))
            st.append(T("dve", I("scalar_tensor_tensor", out=qd[:, :, :], in0=G.qT[:, :, tsl], scalar=float(DK) ** -0.5, in1=e1[:, :, :],
                                 op0=ALU.mult, op1=ALU.mult), G.qT_bs + [e1_b], [qd_b]))
            st.append(T("pool", I("tensor_tensor", out=ki[:, :, :], in0=G.kT[:, :, tsl], in1=e2[:, :, :], op=ALU.mult), G.kT_bs + [e2_b], [ki_b]))
            st.append(T("dve", I("tensor_tensor", out=kd[:, :], in0=G.ktm[:, tt, :], in1=esuf[:, :], op=ALU.mult), G.ktm_bs + [esuf_b], [kd_b]))
            st.append(T("pe", [I("matmul", C.ps[:, 3, 0:128], ki[:, c, :], qd[:, c, :], start=(c == 0), stop=(c == 1)) for c in range(2)],
                        [ki_b, qd_b], [C.ps_b[3]]))
            st.append(T("dve", I("tensor_tensor", out=sT[:, :], in0=C.ps[:, 3, 0:128], in1=C.tri, op=ALU.mult), [C.ps_b[3], C.const_b], [sT_b]))
            return st

        def phaseB(tt, i):
            tsl = slice(tt * 128, (tt + 1) * 128)
            etot, etot_b = G.etot[i], G.etot_b[i]
            qd, qd_b, kd, kd_b = G.qd[i], G.qd_b[i], G.kd[i], G.kd_b[i]
            sT, sT_b = G.sT[i], G.sT_b[i]
            o, o_b, sq, sq_b = G.o[i], G.o_b[i], G.sq[i], G.sq_b[i]
            rstd, rstd_b = G.rstd[i], G.rstd_b[i]
            ops4 = C.ps[:, 4, :].rearrange("p (c t) -> p c t", c=4)
            st = []
            fns = []
            for vc in range(4):
                fns.append(I("matmul", C.ps[:, 4, vc * 128:(vc + 1) * 128], G.vtm[:, tt, vc * 128:(vc + 1) * 128], sT[:, :], start=True, stop=False))
                for c in range(2):
                    fns.append(I("matmul", C.ps[:, 4, vc * 128:(vc + 1) * 128], G.state16[:, c, vc * 128:(vc + 1) * 128], qd[:, c, :],
                                 start=False, stop=(c == 1)))
            st.append(T("pe", fns, G.vtm_bs + [sT_b, G.state16_b, qd_b], [C.ps_b[4]]))
            st.append(T("pe", [I("matmul", C.ps[:, 5 + c, :], kd[:, c * 128:(c + 1) * 128], G.vtm[:, tt, :], start=True, stop=True) for c in range(2)],
                        [kd_b] + G.vtm_bs, [C.ps_b[5], C.ps_b[6]]))
            for c in range(2):
                st.append(T("dve", I("scalar_tensor_tensor", out=G.state[:, c, :], in0=G.state[:, c, :], scalar=etot[:, c:c + 1], in1=C.ps[:, 5 + c, :],
                                     op0=ALU.mult, op1=ALU.add), [etot_b, C.ps_b[5 + c], G.state_b], [G.state_b]))
            st.append(T("pool", I("tensor_copy", out=G.state16[:, :, :], in_=G.state[:, :, :]), [G.state_b], [G.state16_b]))
            st.append(T("act", I("activation", out=o[:, :, :], in_=ops4, func=AF.Copy), [C.ps_b[4]], [o_b]))
            st.append(T("act", I("activation", out=sq[:, :, :], in_=ops4, func=AF.Square), [C.ps_b[4]], [sq_b]))
            st.append(T("pe", [I("matmul", C.ps[:, 7, 0:128], C.ones_bf[:, :], sq[:, vc, :], start=(vc == 0), stop=(vc == 3)) for vc in range(4)],
                        [sq_b, C.const_b], [C.ps_b[7]]))
            st.append(T("act", I("activation", out=rstd[:, :], in_=C.ps[:, 7, 0:128], func=AF.Ln, bias=C.epsc[:, 0:1], scale=1.0 / DV),
                        [C.ps_b[7], C.const_b], [rstd_b]))
            st.append(T("act", I("activation", out=rstd[:, :], in_=rstd[:, :], func=AF.Exp, scale=-0.5), [rstd_b], [rstd_b]))
            for vc in range(4):
                st.append(T("dve", I("scalar_tensor_tensor", out=o[:, vc, :], in0=o[:, vc, :], scalar=G.gn[:, h * 4 + vc:h * 4 + vc + 1], in1=rstd[:, :],
                                     op0=ALU.mult, op1=ALU.mult), [o_b, rstd_b, G.gn_b], [o_b]))
            st.append(T("pool", I("tensor_tensor", out=G.y[:, :, tsl], in0=o[:, :, :], in1=G.gT[:, :, tsl], op=ALU.mult), [o_b] + G.gT_bs, G.y_bs))
            return st

        NTT = S // 128
        idx = [(G.cnt + k) % NT for k in range(NTT)]
        G.cnt += NTT
        for th in phaseA(0, idx[0]):
            th()
        for tt in range(NTT):
            a = phaseA(tt + 1, idx[tt + 1]) if tt + 1 < NTT else []
            b = phaseB(tt, idx[tt])
            for k in range(max(len(a), len(b))):
                if k < len(b):
                    b[k]()
                if k < len(a):
                    a[k]()
            if side is not None:
                next(side, None)
        for vc in range(4):
            P.dma("sp", I("dma_start", out=C.YT[16 + h * 4 + vc, :, :], in_=G.y[:, vc, :]), R=G.y_bs, W=[C.YT_b[16 + h * 4 + vc]])


def stage_gemm_resid(P, C, _unused, w_l, kpasses, src):
    w_view = w_l.rearrange("(c p) n -> p c n", p=128)
    srcT = C.YT if src == "YT" else C.HT
    src_b = C.YT_b if src == "YT" else C.HT_b
    for half in range(NHALF):
        t0 = half * TH
        for pi, (c0, kc) in enumerate(kpasses):
            lastp = (pi == len(kpasses) - 1)
            for c in range(kc):
                P.dma("sp", I("dma_start", out=C.res[:, c, :], in_=srcT[c0 + c, :, t0:t0 + TH]), R=[src_b[c0 + c]], W=[C.res_b[c]])
            C.res_fresh = True
            pending = []
            sbs = [C.ps_b[6], C.ps_b[7]]
            for n0 in range(0, D, 256):
                wb, wi = load_w(P, C, w_view, c0, kc, n0, 256)
                for j in range(2):
                    nch = n0 // 128 + j
                    slot = C.pcnt % (3 if lastp else 4)
                    C.pcnt += 1
                    b0, pbs = mm_chunk(P, C, wb, C.wb_b[wi][j], j * 128, 128, kc, slot)
                    while len(pending) > 2:
                        pending.pop(0)()
                    xs, xs_b = C.next_f32()
                    P.dma("sp", I("dma_start", out=xs[:, :], in_=C.xT[nch, :, t0:t0 + TH]), R=[C.xT_b[nch]], W=[xs_b])
                    P.op("dve", I("tensor_tensor", out=xs[:, :].rearrange("p (a b) -> p a b", a=2), in0=xs[:, :].rearrange("p (a b) -> p a b", a=2),
                                                                         in1=C.ps[:, b0:b0 + 2, :], op=ALU.add), R=pbs + [xs_b], W=[xs_b])
                    P.dma("sp", I("dma_start", out=C.xT[nch, :, t0:t0 + TH], in_=xs[:, :]), R=[xs_b], W=[C.xT_b[nch]])
                    if lastp:
                        sq, sq_b = C.next_b16()
                        P.op("act", I("activation", out=sq[:, :], in_=xs[:, :], func=AF.Square), R=[xs_b], W=[sq_b])
                        fns = [I("matmul", C.ps[:, 6 + t, :], C.ones_bf[:, :], sq[:, t * 512:(t + 1) * 512],
                                 start=(nch == 0), stop=(nch == KC - 1)) for t in range(2)]
                        pending.append(lambda fns=fns, sq_b=sq_b: P.op("pe", fns, R=[sq_b, C.const_b], W=sbs))
            if lastp:
                while pending:
                    pending.pop(0)()
                rs, rs_b = C.rsh[half], C.rsh_b[half]
                P.op("act", I("activation", out=rs[:, :].rearrange("p (a b) -> p a b", a=2), in_=C.ps[:, 6:8, :], func=AF.Sqrt,
                              bias=C.epsc[:, 0:1], scale=1.0 / D), R=sbs + [C.const_b], W=[rs_b])
                P.op("dve", I("reciprocal", out=rs[:, :], in_=rs[:, :]), R=[rs_b], W=[rs_b])
                C.have_stats[half] = True


def stage_gateup(P, C, wg_l, wu_l):
    wg_view = wg_l.rearrange("(c p) n -> p c n", p=128)
    wu_view = wu_l.rearrange("(c p) n -> p c n", p=128)
    for half in range(NHALF):
        t0 = half * TH
        rs, rs_b = norm_to_res(P, C, half, 1)
        norm_apply(P, C, half, 1, rs, rs_b)
        C.res_fresh = True
        for j in range(DFF // 128):
            wb, wi = load_w(P, C, wg_view, 0, KC, j * 128, 128, sub=0)
            wb, wi = load_w(P, C, wu_view, 0, KC, j * 128, 128, sub=1)
            slot = C.pcnt % 4
            C.pcnt += 1
            bg, pg = mm_chunk(P, C, wb, C.wb_b[wi][0], 0, 128, KC, slot)
            slot = C.pcnt % 4
            C.pcnt += 1
            bu, pu = mm_chunk(P, C, wb, C.wb_b[wi][1], 128, 128, KC, slot)
            sg, sg_b = C.next_f32()
            P.op("act", I("activation", out=sg[:, :].rearrange("p (a b) -> p a b", a=2), in_=C.ps[:, bg:bg + 2, :], func=AF.Silu), R=pg, W=[sg_b])
            hb, hb_b = C.next_b16()
            P.op("dve", I("tensor_tensor", out=hb[:, :].rearrange("p (a b) -> p a b", a=2), in0=sg[:, :].rearrange("p (a b) -> p a b", a=2),
                                                                        in1=C.ps[:, bu:bu + 2, :], op=ALU.mult), R=pu + [sg_b], W=[hb_b])
            P.dma("sp", I("dma_start", out=C.HT[j, :, t0:t0 + TH], in_=hb[:, :]), R=[hb_b], W=[C.HT_b[j]])


def stage_final(P, C, out):
    for half in range(NHALF):
        t0 = half * TH
        rs, rs_b = norm_to_res(P, C, half, 2)
        for c in range(KC):
            xs, xs_b = C.next_f32()
            P.dma("sp", I("dma_start", out=xs[:, :], in_=C.xT[c, :, t0:t0 + TH]), R=[C.xT_b[c]], W=[xs_b])
            P.op("dve", I("scalar_tensor_tensor", out=xs[:, :], in0=xs[:, :], scalar=C.gains[:, 2, c:c + 1], in1=rs[:, :], op0=ALU.mult, op1=ALU.mult),
                 R=[xs_b, rs_b, C.gains_b], W=[xs_b])
            b0 = (C.pcnt % 4) * 2
            C.pcnt += 1
            pbs = [C.ps_b[b0], C.ps_b[b0 + 1]]
            fns = [I("transpose", C.ps[:, b0 + k // 4, (k % 4) * 128:(k % 4 + 1) * 128], xs[:, k * 128:(k + 1) * 128], C.ident)
                   for k in range(8)]
            P.op("pe", fns, R=[xs_b, C.const_b], W=pbs)
            ot, ot_b = C.next_f32()
            if c % 2 == 0:
                P.op("act", I("activation", out=ot[:, :].rearrange("p (a b) -> p a b", a=2), in_=C.ps[:, b0:b0 + 2, :], func=AF.Copy), R=pbs, W=[ot_b])
            else:
                P.op("dve", I("tensor_copy", out=ot[:, :].rearrange("p (a b) -> p a b", a=2), in_=C.ps[:, b0:b0 + 2, :]), R=pbs, W=[ot_b])
            P.dma("pool", I("dma_start", out=out[t0:t0 + TH, c * 128:(c + 1) * 128].rearrange("(a p) n -> p a n", p=128),
                                                         in_=ot[:, :].rearrange("p (a n) -> p a n", a=8)), R=[ot_b], is_output=True)


_CACHE = {}


def _get_prog(key):
    if key not in _CACHE:
        _CACHE[key] = build_program(*key)
    return _CACHE[key]


NCORES = 8
NBATCH = 1


def _consts():
    c = np.zeros((128, 3, 128), np.float32)
    p = np.arange(128)[:, None]
    t = np.arange(128)[None, :]
    c[:, 0, :] = (p == t)
    c[:, 1, :] = (p <= t)
    c[:, 2, :] = (p > t)
    return c


def kernel(x, mix_norm, w_in, conv_w, gla_a_up, gla_a_bias, gla_norm, w_out, ffn_norm, w_gate, w_up, w_down, final_norm):
    x = np.asarray(x, dtype=np.float32)
    B = x.shape[0]
    assert B == NCORES * NBATCH
    nc = _get_prog((tuple(range(DEPTH)), True, True, DEPTH, NBATCH))
    shared = {
        "mix_norm": np.asarray(mix_norm, np.float32), "w_in": np.asarray(w_in, np.float32),
        "conv_w": np.asarray(conv_w, np.float32), "gla_a_up": np.asarray(gla_a_up, np.float32),
        "gla_a_bias": np.asarray(gla_a_bias, np.float32).reshape(DEPTH, 1, KEYW),
        "gla_norm": np.asarray(gla_norm, np.float32).reshape(DEPTH, 1, GLAW),
        "w_out": np.asarray(w_out, np.float32), "ffn_norm": np.asarray(ffn_norm, np.float32),
        "w_gate": np.asarray(w_gate, np.float32), "w_up": np.asarray(w_up, np.float32),
        "w_down": np.asarray(w_down, np.float32), "final_norm": np.asarray(final_norm, np.float32).reshape(1, D),
        "consts": _consts(),
    }
    in_maps = [dict(shared, x=x[c * NBATCH:(c + 1) * NBATCH]) for c in range(NCORES)]
    res = run_bass_kernel_spmd(nc, in_maps, core_ids=list(range(NCORES)))
    return np.concatenate([np.asarray(r["out"], dtype=np.float32) for r in res.results], axis=0)
```
